# Optimizing a Trainium2 kernel written in Bass

```python
import math
import jax, jax.numpy as jnp
from jax import lax
import numpy as np

D_MODEL = 2048
BATCH = 4
SEQ = 2048
DEPTH = 4

GRID_W = 64
CTX_LEN = 256
D_MIX = D_MODEL
D_MLSTM = D_MIX // 2
MLSTM_HEADS = 4
MLSTM_DV = D_MLSTM // MLSTM_HEADS
MLSTM_DQK = MLSTM_DV // 2
CHUNK = 64
D_CONV = D_MIX - D_MLSTM
CONV_GROUPS = 8
CONV_GROUP_W = D_CONV // CONV_GROUPS
D_CONV_H = (CONV_GROUPS // 2) * CONV_GROUP_W
CONV_W = 3
N_EXPERTS = 16
N_GROUPS = 4
EXPERTS_PER_GROUP = N_EXPERTS // N_GROUPS
TOP_K = 2
D_FF = D_MODEL // 2
ALPHA = (2 * DEPTH) ** 0.25
BETA = (8 * DEPTH) ** -0.25
LN_EPS = 1e-6
SPLIT_SIZES = (MLSTM_HEADS * MLSTM_DQK, MLSTM_HEADS * MLSTM_DQK, D_MLSTM, 2 * MLSTM_HEADS, 2 * MLSTM_HEADS,
               D_MLSTM, D_CONV, D_CONV, D_CONV)
D_STATE_COLS = 2 * MLSTM_HEADS * MLSTM_DQK + D_MLSTM + 4 * MLSTM_HEADS
D_IN = D_STATE_COLS + D_MLSTM + 3 * D_CONV

kernel_name = "hybrid_mlstm_shortconv_moe_dit"


def _split(u, sizes):
    return jnp.split(u, np.cumsum(sizes)[:-1].tolist(), axis=-1)


def _ln(x):
    xf = x.astype(jnp.float32)
    xc = xf - xf.mean(-1, keepdims=True)
    return xc * lax.rsqrt((xc * xc).mean(-1, keepdims=True) + LN_EPS)


def _modulate(x, shift, scale):
    return (_ln(x) * (1 + scale) + shift).astype(x.dtype)


def _post_norm(x, g, b):
    return (_ln(x) * g + b).astype(x.dtype)


def _mlstm_prep(q, k, v, ig, fg, b_i, b_f):
    Bn, T, _ = q.shape
    heads = lambda a: a.reshape(Bn, T, MLSTM_HEADS, -1).transpose(0, 2, 1, 3).astype(jnp.float32)
    gates = lambda a: a.astype(jnp.float32).reshape(Bn, T, 2, MLSTM_HEADS).transpose(2, 0, 3, 1)
    return (heads(q), heads(k) * MLSTM_DQK ** -0.5, heads(v),
            gates(ig + b_i), jax.nn.log_sigmoid(gates(fg + b_f)))


def _mlstm_scan(q, k, v, ig, lf, state):
    Bn, H, T, _ = q.shape
    nc = T // CHUNK

    def to_chunks(a):
        return jnp.moveaxis(a.reshape(a.shape[:2] + (nc, CHUNK) + a.shape[3:]), 2, 0)

    lower = jnp.tril(jnp.ones((CHUNK, CHUNK), dtype=bool))

    def step(carry, inp):
        C, n, m = carry
        qc, kc, vc, ic, fc = inp
        b = jnp.cumsum(fc, axis=-1)
        dmat = b[..., :, None] - b[..., None, :] + ic[..., None, :]
        dmat = jnp.where(lower, dmat, -jnp.inf)
        m_t = jnp.maximum(b + m[..., None], dmat.max(-1))
        inter = jnp.exp(b + m[..., None] - m_t)
        w = jnp.exp(dmat - m_t[..., None])
        s = jnp.einsum('bhtk,bhsk->bhts', qc, kc) * w
        num = inter[..., None] * jnp.einsum('bhvk,bhtk->bhtv', C, qc) + jnp.einsum('bhts,bhsv->bhtv', s, vc)
        den = inter * jnp.einsum('bhk,bhtk->bht', n, qc) + s.sum(-1)
        h = num / jnp.maximum(jnp.abs(den), jnp.exp(-m_t))[..., None]
        decay = inter[..., -1]
        wg = w[..., -1, :]
        C = decay[..., None, None] * C + jnp.einsum('bhs,bhsv,bhsk->bhvk', wg, vc, kc)
        n = decay[..., None] * n + jnp.einsum('bhs,bhsk->bhk', wg, kc)
        return (C, n, m_t[..., -1]), h

    state, h = lax.scan(step, state, tuple(to_chunks(a) for a in (q, k, v, ig, lf)))
    return jnp.moveaxis(h, 0, 2).reshape(Bn, H, T, -1), state


def _mlstm_bidir(ctx_in, lat_in):
    qc, kc, vc, ic, fc = ctx_in
    ql, kl, vl, il, fl = lat_in
    Bn = qc.shape[0]
    zero = (jnp.zeros((Bn, MLSTM_HEADS, MLSTM_DV, MLSTM_DQK), jnp.float32),
            jnp.zeros((Bn, MLSTM_HEADS, MLSTM_DQK), jnp.float32),
            jnp.zeros((Bn, MLSTM_HEADS), jnp.float32))
    rev = lambda a: jnp.flip(a, axis=2)
    hcf, st_f = _mlstm_scan(qc, kc, vc, ic[0], fc[0], zero)
    hcb, st_b = _mlstm_scan(rev(qc), rev(kc), rev(vc), rev(ic[1]), rev(fc[1]), zero)
    hlf, _ = _mlstm_scan(ql, kl, vl, il[0], fl[0], st_f)
    hlb, _ = _mlstm_scan(rev(ql), rev(kl), rev(vl), rev(il[1]), rev(fl[1]), st_b)
    return hcf + rev(hcb), hlf + rev(hlb)


def _mlstm_out(h, o, g):
    Bn, H, T, DV = h.shape
    hn = h * lax.rsqrt(jnp.mean(h * h, -1, keepdims=True) + LN_EPS)
    hn = hn.transpose(0, 2, 1, 3).reshape(Bn, T, H * DV)
    return (hn * g * jax.nn.sigmoid(o.astype(jnp.float32))).astype(o.dtype)


def _conv3(z, w, axis):
    n = z.shape[axis]
    pad = [(0, 0)] * z.ndim
    pad[axis] = (1, 1)
    zp = jnp.pad(z, pad)
    sl = lambda s: lax.slice_in_dim(zp, s, s + n, axis=axis)
    return w[0] * sl(0) + w[1] * sl(1) + w[2] * sl(2)


def _conv_latent(z, w, rows):
    Bn, S, Cd = z.shape
    zg = z.reshape(Bn, rows, GRID_W, Cd)
    yh = _conv3(zg[..., :D_CONV_H], w[:, :D_CONV_H], axis=2)
    yv = _conv3(zg[..., D_CONV_H:], w[:, D_CONV_H:], axis=1)
    return jnp.concatenate([yh, yv], -1).reshape(Bn, S, Cd)


def _moe(h, w_router, b_router, w1, w3, w2):
    s = jax.nn.sigmoid((h @ w_router).astype(jnp.float32))
    sb = s + b_router.astype(jnp.float32)
    gscore = lax.top_k(sb.reshape(-1, N_GROUPS, EXPERTS_PER_GROUP), 2)[0].sum(-1)
    gsel = jnp.argmax(gscore, axis=-1)
    in_group = (jnp.arange(N_EXPERTS) // EXPERTS_PER_GROUP)[None, :] == gsel[:, None]
    _, idx = lax.top_k(jnp.where(in_group, sb, -jnp.inf), TOP_K)
    sel = jnp.take_along_axis(s, idx, axis=-1)
    wts = sel / sel.sum(-1, keepdims=True)
    gates = (jax.nn.one_hot(idx, N_EXPERTS, dtype=jnp.float32) * wts[..., None]).sum(1).astype(h.dtype)
    out = jnp.zeros_like(h)
    for e in range(N_EXPERTS):
        a = jax.nn.silu(h @ w1[e]) * (h @ w3[e])
        out = out + gates[:, e:e + 1] * (a @ w2[e])
    return out


def setup_inputs(seed: int = 0) -> dict:
    key = jax.random.key(seed)
    ks = jax.random.split(key, 24)
    f32 = jnp.float32
    nrm = lambda k, shape, scale: jax.random.normal(k, shape, f32) * scale
    lin = jnp.linspace(3.0, 6.0, MLSTM_HEADS, dtype=f32)
    return {
        "x": nrm(ks[0], (BATCH, SEQ, D_MODEL), 1.0),
        "c": nrm(ks[1], (BATCH, D_MODEL), 1.0),
        "ctx": nrm(ks[2], (BATCH, CTX_LEN, D_MODEL), 1.0),
        "c_ctx": nrm(ks[3], (D_MODEL,), 1.0),
        "w_ada": nrm(ks[4], (DEPTH, D_MODEL, 6 * D_MODEL), 0.5 * D_MODEL ** -0.5),
        "b_ada": nrm(ks[5], (DEPTH, 6 * D_MODEL), 0.02),
        "w_in": nrm(ks[6], (DEPTH, D_MODEL, D_IN), D_MODEL ** -0.5),
        "b_igate": nrm(ks[7], (DEPTH, 2 * MLSTM_HEADS), 0.1),
        "b_fgate": jnp.concatenate([lin, lin])[None, :] + nrm(ks[8], (DEPTH, 2 * MLSTM_HEADS), 0.1),
        "mh_norm_g": 1.0 + nrm(ks[9], (DEPTH, D_MLSTM), 0.02),
        "conv_w": nrm(ks[10], (DEPTH, CONV_W, D_CONV), CONV_W ** -0.5),
        "conv_b": nrm(ks[11], (DEPTH, D_CONV), 0.02),
        "w_out": nrm(ks[12], (DEPTH, D_MIX, D_MODEL), BETA * D_MIX ** -0.5),
        "ln1_g": 1.0 + nrm(ks[13], (DEPTH, D_MODEL), 0.02),
        "ln1_b": nrm(ks[14], (DEPTH, D_MODEL), 0.02),
        "w_router": nrm(ks[15], (D_MODEL, N_EXPERTS), D_MODEL ** -0.5),
        "b_router": nrm(ks[16], (N_EXPERTS,), 0.01),
        "w1": nrm(ks[17], (DEPTH, N_EXPERTS, D_MODEL, D_FF), D_MODEL ** -0.5),
        "w3": nrm(ks[18], (DEPTH, N_EXPERTS, D_MODEL, D_FF), D_MODEL ** -0.5),
        "w2": nrm(ks[19], (DEPTH, N_EXPERTS, D_FF, D_MODEL), BETA * D_FF ** -0.5),
        "ln2_g": 1.0 + nrm(ks[20], (DEPTH, D_MODEL), 0.02),
        "ln2_b": nrm(ks[21], (DEPTH, D_MODEL), 0.02),
    }


def reference(x, c, ctx, c_ctx, w_ada, b_ada, w_in, b_igate, b_fgate, mh_norm_g, conv_w, conv_b,
              w_out, ln1_g, ln1_b, w_router, b_router, w1, w3, w2, ln2_g, ln2_b):
    rows = x.shape[1] // GRID_W
    cond = jax.nn.silu(jnp.concatenate([c, c_ctx[None, :]], axis=0))
    xl, xc = x, ctx
    for l in range(DEPTH):
        last = l == DEPTH - 1
        mod = (cond @ w_ada[l] + b_ada[l]).reshape(cond.shape[0], 6, 1, D_MODEL)
        ml, mc = mod[:-1], mod[-1:]

        hl = _modulate(xl, ml[:, 0], ml[:, 1])
        hc = _modulate(xc, mc[:, 0], mc[:, 1])
        ql, kl, vl, il, fl, ol, ul, bgl, cgl = _split(hl @ w_in[l], SPLIT_SIZES)
        if last:
            pc = _split(hc @ w_in[l][:, :D_STATE_COLS], SPLIT_SIZES[:5])
        else:
            pc = _split(hc @ w_in[l], SPLIT_SIZES)
        ctx_m = _mlstm_prep(pc[0], pc[1], pc[2], pc[3], pc[4], b_igate[l], b_fgate[l])
        lat_m = _mlstm_prep(ql, kl, vl, il, fl, b_igate[l], b_fgate[l])
        h_ctx, h_lat = _mlstm_bidir(ctx_m, lat_m)
        m_lat = _mlstm_out(h_lat, ol, mh_norm_g[l])
        y_lat = bgl * (_conv_latent(cgl * ul, conv_w[l], rows) + conv_b[l])
        out_l = jnp.concatenate([m_lat, y_lat], axis=-1) @ w_out[l]
        xl = _post_norm(ALPHA * xl + ml[:, 2] * out_l, ln1_g[l], ln1_b[l])
        if not last:
            oc, uc, bgc, cgc = pc[5], pc[6], pc[7], pc[8]
            m_ctx = _mlstm_out(h_ctx, oc, mh_norm_g[l])
            y_ctx = bgc * (_conv3(cgc * uc, conv_w[l], axis=1) + conv_b[l])
            out_c = jnp.concatenate([m_ctx, y_ctx], axis=-1) @ w_out[l]
            xc = _post_norm(ALPHA * xc + mc[:, 2] * out_c, ln1_g[l], ln1_b[l])

        hl = _modulate(xl, ml[:, 3], ml[:, 4]).reshape(-1, D_MODEL)
        n_lat = hl.shape[0]
        if last:
            tokens = hl
        else:
            hc = _modulate(xc, mc[:, 3], mc[:, 4]).reshape(-1, D_MODEL)
            tokens = jnp.concatenate([hl, hc], axis=0)
        moe = _moe(tokens, w_router, b_router, w1[l], w3[l], w2[l])
        xl = _post_norm(ALPHA * xl + ml[:, 5] * moe[:n_lat].reshape(xl.shape), ln2_g[l], ln2_b[l])
        if not last:
            xc = _post_norm(ALPHA * xc + mc[:, 5] * moe[n_lat:].reshape(xc.shape), ln2_g[l], ln2_b[l])
    return xl
```

```python
import numpy as np
from contextlib import ExitStack
import concourse.bass as bass
import concourse.mybir as mybir
from concourse.bass_utils import run_bass_kernel_spmd

F32 = mybir.dt.float32
BF16 = mybir.dt.bfloat16
AF = mybir.ActivationFunctionType
ALU = mybir.AluOpType
AX = mybir.AxisListType

P = 128
D = 2048
KC = 16
T = 2304
NT = 18
TH = 1152
NTH = 9
NE = 16
FF = 1024
DIN = 6160
ALPHA = 8.0 ** 0.25
EPS = 1e-6
RG = [[0, 1], [2, 3], [4, 5], [6, 7]]
ORDER = [list(range(18)), [1, 0] + list(range(17, 1, -1))]
SAME_ENGINE_SYNC = True


class Tile:
    __slots__ = ("name", "w", "r", "ds", "excl")

    def __init__(self, name, excl=False):
        self.name = name
        self.w = None
        self.r = {}
        self.ds = None
        self.excl = excl


class _DS:
    def __init__(self, name, sem):
        self.name = name
        self.sem = sem
        self.cnt = 0


class _Px:
    def __init__(self, s, e, r, w, sig):
        self.s, self.e, self.r, self.w, self.sig = s, e, r, w, sig

    def __getattr__(self, name):
        def f(*a, **k):
            self.s._pre(self.e, self.r, self.w)
            ins = getattr(self.s.eng[self.e], name)(*a, **k)
            self.s._post(self.e, ins, self.r, self.w, self.sig)
            return ins
        return f


class Sched:
    def __init__(self, nc, st, n_dsem=56):
        self.nc = nc
        self.eng = {"pe": nc.tensor, "act": nc.scalar, "dve": nc.vector, "pool": nc.gpsimd, "sp": nc.sync}
        self.sem = {k: st.enter_context(nc.semaphore("s_" + k)) for k in self.eng}
        self.cnt = {k: 0 for k in self.eng}
        self.seen = {k: {} for k in self.eng}
        self.dsp = [_DS("d%d" % i, st.enter_context(nc.semaphore("d%d" % i))) for i in range(n_dsem)]
        self.dsi = 0
        self.extra = []

    def _pre(self, e, r, w):
        deps = {}

        def add(d):
            if d is None:
                return
            k, sem, v = d
            if k not in deps or deps[k][1] < v:
                deps[k] = (sem, v)
        for t in r:
            add(t.w)
        for t in w:
            add(t.w)
            for d in t.r.values():
                add(d)
        eng = self.eng[e]
        seen = self.seen[e]
        for k, (sem, v) in deps.items():
            if k == e and (e == "pe" or not SAME_ENGINE_SYNC):
                continue
            if seen.get(k, 0) >= v:
                continue
            if k in self.cnt:
                assert v <= self.cnt[k], "dependency on unsignalled instruction %s %d>%d" % (k, v, self.cnt[k])
            eng.wait_ge(sem, v)
            seen[k] = v

    def _post(self, e, ins, r, w, sig):
        if sig:
            self.cnt[e] += 1
            ins.then_inc(self.sem[e], 1)
            v = self.cnt[e]
        else:
            v = self.cnt[e] + 1
        d = (e, self.sem[e], v)
        for t in r:
            t.r[e] = d
        for t in w:
            t.w = d
            t.r = {}

    def op(self, e, r=(), w=(), sig=True):
        r, w = list(r), list(w)
        ex = [t for t in r if t.excl]
        if ex:
            r = [t for t in r if not t.excl]
            w = w + [t for t in ex if t not in w]
        return _Px(self, e, r, w, sig)

    def pe(self, r=(), w=(), sig=True):
        return self.op("pe", r, w, sig)

    def act(self, r=(), w=()):
        return self.op("act", r, w)

    def dve(self, r=(), w=()):
        return self.op("dve", r, w)

    def pool(self, r=(), w=()):
        return self.op("pool", r, w)

    def dma(self, q, out, in_, r=(), w=()):
        r, w = list(r), list(w)
        self._pre(q, r, w)
        owner = w[0] if w else r[0]
        if owner.ds is None:
            owner.ds = self.dsp[self.dsi % len(self.dsp)]
            self.dsi += 1
        ds = owner.ds
        ins = self.eng[q].dma_start(out=out, in_=in_)
        ds.cnt += 16
        ins.then_inc(ds.sem, 16)
        d = (ds.name, ds.sem, ds.cnt)
        for t in r:
            t.r[ds.name] = d
        for t in w:
            t.w = d
            t.r = {}
        return ins

    def barrier(self):
        evs = [(k, self.sem[k], self.cnt[k]) for k in self.eng if self.cnt[k] > 0]
        evs += [(ds.name, ds.sem, ds.cnt) for ds in self.dsp if ds.cnt > 0]
        evs += self.extra
        for e, eng in self.eng.items():
            seen = self.seen[e]
            for k, sem, v in evs:
                if seen.get(k, 0) >= v:
                    continue
                eng.wait_ge(sem, v)
                seen[k] = v


class Buf:
    def __init__(self, t, name):
        self.h = t
        self.ap = t[:]
        self.t = Tile(name)


class Ring:
    def __init__(self, bufs):
        self.bufs = bufs
        self.i = 0

    def next(self):
        b = self.bufs[self.i % len(self.bufs)]
        self.i += 1
        return b


def build(depth=4, stop_after=None, debug=False):
    nc = bass.Bass("TRN2", target_bir_lowering=False)

    def din(name, shape, dt=F32):
        return nc.dram_tensor(name, shape, dt, kind="ExternalInput")

    def dscr(name, shape, dt=F32, dump=False):
        if debug and dump:
            return nc.dram_tensor(name, shape, dt, kind="ExternalOutput")
        return nc.dram_tensor(name, shape, dt)

    x_in = din("x_in", [T, D])
    cond5 = din("cond5", [P, KC, 5])
    w_ada_sh = din("w_ada_sh", [D, depth * 1536])
    b_ada_sh = din("b_ada_sh", [P, depth * 12])
    selb = din("selb", [P, 4])
    w_in = din("w_in", [depth, D, DIN])
    bgate = din("bgate", [depth, 16])
    mhg = din("mhg", [depth, 1024])
    conv_w = din("conv_w", [depth, P, 3, 8])
    conv_b = din("conv_b", [depth, P, 8])
    w_out = din("w_out", [depth, D, D])
    ln1_g = din("ln1_g", [depth, D])
    ln1_b = din("ln1_b", [depth, D])
    ln2_g = din("ln2_g", [depth, D])
    ln2_b = din("ln2_b", [depth, D])
    w_router = din("w_router", [P, KC, 16])
    b_router = din("b_router", [1, 16])
    w1 = din("w1", [depth, NE, D, FF])
    w3 = din("w3", [depth, NE, D, FF])
    w2 = din("w2", [depth, NE, FF, D])
    ident = din("ident", [P, P])
    masks = din("masks", [2, P, P])
    sel = din("sel", [P, 2])
    out = nc.dram_tensor("out", [2048, D], F32, kind="ExternalOutput")

    xl_d = dscr("xl_d", [T, D], F32, True)
    ccm_in = nc.dram_tensor("ccm_in", [P, depth * 60], F32)
    ccm_out = nc.dram_tensor("ccm_out", [8 * P, depth * 60], F32)
    qT_d = dscr("qT_d", [4, P, T], BF16, True)
    kT_d = dscr("kT_d", [4, P, T], BF16, True)
    k_d = dscr("k_d", [T, 512], BF16, True)
    v_d = dscr("v_d", [T, 1024], BF16, True)
    gates_d = dscr("gates_d", [T, 16], F32, True)
    so_d = dscr("so_d", [T, 1024], BF16, True)
    mixT_d = dscr("mixT_d", [16, P, T], BF16, True)
    h2T_d = dscr("h2T_d", [P, KC, T], BF16, True)
    logit_d = dscr("logit_d", [T, 16], F32, True)
    cc_in = [nc.dram_tensor("cc_in%d" % i, [P, D], F32) for i in range(NTH)]
    cc_out = [nc.dram_tensor("cc_out%d" % i, [2 * P, D], F32) for i in range(NTH)]
    dbg_mod = dscr("dbg_mod", [P, depth * 4 * 16 * 2], F32, True)
    dbg_moe = dscr("dbg_moe", [TH, D], F32, True)
    dbg_gates = dscr("dbg_gates", [P, NTH * 16], F32, True)

    with ExitStack() as st:
        S = Sched(nc, st)
        cc_sem = st.enter_context(nc.semaphore("cc_sem"))
        cc_n = [0]

        uid = [0]

        def sbuf(stk, name, shape, dt):
            uid[0] += 1
            return stk.enter_context(nc.sbuf_tensor("%s_u%d" % (name, uid[0]), shape, dt))

        def mk(stk, name, shape, dt):
            return Buf(sbuf(stk, name, shape, dt), name)

        def ring(stk, name, shape, dt, n):
            return Ring([mk(stk, "%s%d" % (name, i), shape, dt) for i in range(n)])

        banks = [Buf(st.enter_context(nc.psum_tensor("ps%d" % i, [P, 512], F32)), "ps%d" % i) for i in range(8)]
        for b_ in banks:
            b_.t.excl = True
        bank_i = [0]

        def bank():
            b = banks[bank_i[0] % 8]
            bank_i[0] += 1
            return b

        idf = mk(st, "idf", [P, P], F32)
        idb = mk(st, "idb", [P, P], BF16)
        mkf = [mk(st, "mk0", [P, P], F32), mk(st, "mk1", [P, P], F32)]
        ones_col = mk(st, "ones_col", [P, 1], F32)
        ones_row = mk(st, "ones_row", [1, P], F32)
        selt = mk(st, "selt", [P, 2], F32)
        modT = mk(st, "modT", [P, depth, 4, KC, 2], F32)
        stats_r = ring(st, "stats", [P, 32], F32, 3)

        S.dma("sp", out=idf.ap, in_=ident[:, :], w=[idf.t])
        S.dma("sp", out=mkf[0].ap, in_=masks[0, :, :], w=[mkf[0].t])
        S.dma("sp", out=mkf[1].ap, in_=masks[1, :, :], w=[mkf[1].t])
        S.dma("sp", out=selt.ap, in_=sel[:, :], w=[selt.t])
        S.dve([idf.t], [idb.t]).tensor_copy(out=idb.ap, in_=idf.ap)
        S.dve([], [ones_col.t]).memset(ones_col.ap, 1.0)
        S.dve([], [ones_row.t]).memset(ones_row.ap, 1.0)

        def finish():
            S.barrier()

        def ln_stats(xap, tx):
            sb_ = stats_r.next()
            a = sb_.ap
            for k in range(4):
                S.dve([tx], [sb_.t]).bn_stats(a[:, k * 6:(k + 1) * 6], xap[:, k * 512:(k + 1) * 512])
            S.dve([sb_.t], [sb_.t]).bn_aggr(a[:, 24:26], a[:, 0:24])
            S.act([sb_.t], [sb_.t]).activation(out=a[:, 26:27], in_=a[:, 25:26], func=AF.Sqrt, bias=EPS, scale=1.0)
            S.dve([sb_.t], [sb_.t]).reciprocal(a[:, 27:28], a[:, 26:27])
            return a[:, 24:25], a[:, 27:28], sb_.t

        def ln_mod_T(xap, tx, xn_ring, l, kind, r, dst_fn):
            mean, rstd, ts = ln_stats(xap, tx)
            xn = xn_ring.next()
            S.dve([tx, ts], [xn.t]).tensor_scalar(out=xn.ap, in0=xap, scalar1=mean, scalar2=rstd,
                                                  op0=ALU.subtract, op1=ALU.mult)
            for half in range(2):
                b = bank()
                pb = b.ap.bitcast(BF16)
                for cc in range(8):
                    c = half * 8 + cc
                    S.pe([xn.t, idb.t], [b.t], sig=(cc == 7)).transpose(
                        out=pb[:, cc * 128:(cc + 1) * 128], in_=xn.ap[:, c * 128:(c + 1) * 128], identity=idb.ap)
                for cc in range(8):
                    c = half * 8 + cc
                    dap, dt_ = dst_fn(c)
                    S.act([b.t, modT.t], [dt_]).activation(
                        out=dap, in_=pb[:, cc * 128:(cc + 1) * 128], func=AF.Identity,
                        scale=modT.ap[:, l, kind + 1, c, r:r + 1], bias=modT.ap[:, l, kind, c, r:r + 1])

        def ln_affine(xt, g_b, b_b):
            mean, rstd, ts = ln_stats(xt.ap, xt.t)
            S.dve([xt.t, ts], [xt.t]).tensor_scalar(out=xt.ap, in0=xt.ap, scalar1=mean, scalar2=rstd,
                                                    op0=ALU.subtract, op1=ALU.mult)
            S.dve([xt.t, g_b.t], [xt.t]).tensor_tensor(out=xt.ap, in0=xt.ap, in1=g_b.ap, op=ALU.mult)
            S.dve([xt.t, b_b.t], [xt.t]).tensor_tensor(out=xt.ap, in0=xt.ap, in1=b_b.ap, op=ALU.add)

        NCH = depth * 12
        NB = NCH // 4
        modb = mk(st, "modb", [P, depth * 96], F32)
        modc = mk(st, "modc", [P, depth * 96], F32)
        ones_f = mk(st, "ones_f", [P, P], F32)
        dg_r = ring(st, "dg_r", [P, P], F32, 3)
        S.dve([], [ones_f.t]).memset(ones_f.ap, 1.0)
        with ExitStack() as ph:
            cond_f = mk(ph, "cond_f", [P, KC, 5], F32)
            cond_s = mk(ph, "cond_s", [P, KC, 5], F32)
            condT = mk(ph, "condT", [P, KC, 5], BF16)
            bsh = mk(ph, "bsh", [P, NCH], F32)
            selbt = mk(ph, "selbt", [P, 4], F32)
            mod_sh = mk(ph, "mod_sh", [P, NCH, 5], F32)
            modall = mk(ph, "modall", [P, 8, NCH * 5], F32)
            wr0 = ring(ph, "w0r", [P, KC, 512], BF16, 3)
            S.dma("sp", out=cond_f.ap, in_=cond5[:, :, :], w=[cond_f.t])
            S.dma("sp", out=bsh.ap, in_=b_ada_sh[:, :], w=[bsh.t])
            S.dma("sp", out=selbt.ap, in_=selb[:, :], w=[selbt.t])
            S.act([cond_f.t], [cond_s.t]).activation(out=cond_s.ap, in_=cond_f.ap, func=AF.Silu)
            S.dve([cond_s.t], [condT.t]).tensor_copy(out=condT.ap, in_=cond_s.ap)
            for blk in range(NB):
                wb = wr0.next()
                S.dma("pool", out=wb.ap, in_=w_ada_sh[:, blk * 512:(blk + 1) * 512].rearrange("(kc p) n -> p kc n", p=P),
                      w=[wb.t])
                for cc in range(4):
                    cq = blk * 4 + cc
                    b = bank()
                    for kc in range(KC):
                        S.pe([wb.t, condT.t], [b.t], sig=(kc == KC - 1)).matmul(
                            b.ap[:, 0:5], wb.ap[:, kc, cc * 128:(cc + 1) * 128], condT.ap[:, kc, :],
                            start=(kc == 0), stop=(kc == KC - 1))
                    S.dve([b.t, bsh.t], [mod_sh.t]).tensor_scalar(
                        out=mod_sh.ap[:, cq, :], in0=b.ap[:, 0:5], scalar1=bsh.ap[:, cq:cq + 1], scalar2=None, op0=ALU.add)
            S.dma("sp", out=ccm_in[:, :], in_=mod_sh.ap.rearrange("p q r -> p (q r)"), r=[mod_sh.t])
            S._pre("pool", [], [mod_sh.t])
            cc_n[0] += 1
            nc.gpsimd.collective_compute("AllGather", ALU.bypass, replica_groups=[list(range(8))],
                                         ins=[ccm_in.ap().opt()], outs=[ccm_out.ap().opt()]).then_inc(cc_sem, 1)
            S.extra = [("cc", cc_sem, cc_n[0])]
            S.barrier()
            S.dma("sp", out=modall.ap, in_=ccm_out.ap().rearrange("(k p) f -> p k f", p=P), w=[modall.t])
            mv = modall.ap.rearrange("p k (q r) -> p (k q) r", r=5)
            S.dve([modall.t, selbt.t], [modb.t]).tensor_scalar(out=modb.ap, in0=mv[:, :, 0], scalar1=selbt.ap[:, 0:1],
                                                               scalar2=None, op0=ALU.mult)
            for bb_ in range(1, 4):
                S.dve([modall.t, selbt.t, modb.t], [modb.t]).scalar_tensor_tensor(
                    out=modb.ap, in0=mv[:, :, bb_], scalar=selbt.ap[:, bb_:bb_ + 1], in1=modb.ap, op0=ALU.mult, op1=ALU.add)
            S.dve([modall.t], [modc.t]).tensor_copy(out=modc.ap, in_=mv[:, :, 4])
            for l in range(depth):
                for kind, j in ((0, 0), (1, 1), (2, 3), (3, 4)):
                    for r, src in ((0, modb), (1, modc)):
                        q0 = l * 96 + j * 16
                        S.dve([src.t], [modT.t]).tensor_scalar(
                            out=modT.ap[:, l, kind, :, r], in0=src.ap[:, q0:q0 + 16],
                            scalar1=(1.0 if j in (1, 4) else 0.0), scalar2=None, op0=ALU.add)
            if debug:
                S.dma("sp", out=dbg_mod[:, :], in_=modT.ap.rearrange("p l k c r -> p (l k c r)"), r=[modT.t])
            S.barrier()
        if stop_after == "M0":
            finish()
            return nc

        def gate_bcast(dst, l, j, r):
            src = modb if r == 0 else modc
            for qd in range(4):
                b = bank()
                for cc in range(4):
                    q = l * 96 + j * 16 + qd * 4 + cc
                    dg = dg_r.next()
                    S.dve([idf.t, src.t], [dg.t]).tensor_scalar(out=dg.ap, in0=idf.ap, scalar1=src.ap[:, q:q + 1],
                                                                scalar2=None, op0=ALU.mult)
                    S.pe([ones_f.t, dg.t], [b.t]).matmul(b.ap[:, cc * 128:(cc + 1) * 128], ones_f.ap, dg.ap,
                                                         start=True, stop=True)
                S.act([b.t], [dst.t]).copy(out=dst.ap[:, qd * 512:(qd + 1) * 512], in_=b.ap)

        for l in range(depth):
            last = (l == depth - 1)
            xsrc = x_in if l == 0 else xl_d

            with ExitStack() as ph:
                hT = sbuf(ph, "hT", [P, KC, T], BF16)
                t_hT = [Tile("hT%d" % i) for i in range(NT)]
                with ExitStack() as ph1:
                    x_r = ring(ph1, "x_r", [P, D], F32, 2)
                    xn_r = ring(ph1, "xn_r", [P, D], BF16, 2)
                    for i in range(NT):
                        r = 1 if i < 2 else 0
                        xt = x_r.next()
                        S.dma("sp", out=xt.ap, in_=xsrc[i * 128:(i + 1) * 128, :], w=[xt.t])
                        ln_mod_T(xt.ap, xt.t, xn_r, l, 0, r,
                                 lambda c, i=i: (hT[:, c, i * 128:(i + 1) * 128], t_hT[i]))
                    S.barrier()
                if stop_after == "M1":
                    S.dma("sp", out=h2T_d[:, :, :], in_=hT[:], r=t_hT)
                    finish()
                    return nc

                wr = ring(ph, "wr", [P, KC, 512], BF16, 3)
                stg_bf = ring(ph, "stg_bf", [P, 512], BF16, 4)
                stg_f = ring(ph, "stg_f", [P, 512], F32, 3)
                gates_sb = mk(ph, "gates_sb", [P, NT, 16], F32)
                bg_b = mk(ph, "bg_b", [P, 16], F32)
                cw = mk(ph, "cw", [P, 3, 8], F32)
                cb = mk(ph, "cb", [P, 8], F32)
                zT = sbuf(ph, "zT", [P, 4, T], BF16)
                bgT = sbuf(ph, "bgT", [P, 4, T], BF16)
                t_z = [Tile("z%d" % i) for i in range(4)]
                t_bg = [Tile("bg%d" % i) for i in range(4)]
                y_r = ring(ph, "y_r", [P, T], F32, 1)
                ym_r = ring(ph, "ym_r", [P, T], BF16, 2)
                S.dma("sp", out=bg_b.ap, in_=bgate[l:l + 1, :].partition_broadcast(P), w=[bg_b.t])
                S.dma("sp", out=cw.ap, in_=conv_w[l, :, :, :], w=[cw.t])
                S.dma("sp", out=cb.ap, in_=conv_b[l, :, :], w=[cb.t])

                TBS = [(0, 512), (512, 512), (1024, 512), (1536, 512), (2048, 256)]

                def load_w(col0, n):
                    wb = wr.next()
                    S.dma("pool", out=wb.ap[:, :, 0:n],
                          in_=w_in[l, :, col0:col0 + n].rearrange("(kc p) n -> p kc n", p=P), w=[wb.t])
                    return wb

                def fm(wb, cc, t0, n):
                    b = bank()
                    rt = [t_hT[i] for i in range(t0 // 128, (t0 + n) // 128)]
                    for kc in range(KC):
                        S.pe([wb.t] + rt, [b.t], sig=(kc == KC - 1)).matmul(
                            b.ap[:, 0:n], wb.ap[:, kc, cc * 128:(cc + 1) * 128], hT[:, kc, t0:t0 + n],
                            start=(kc == 0), stop=(kc == KC - 1))
                    return b

                def tm(wb, i, n):
                    b = bank()
                    for kc in range(KC):
                        S.pe([wb.t, t_hT[i]], [b.t], sig=(kc == KC - 1)).matmul(
                            b.ap[:, 0:n], hT[:, kc, i * 128:(i + 1) * 128], wb.ap[:, kc, 0:n],
                            start=(kc == 0), stop=(kc == KC - 1))
                    return b

                KS = 128.0 ** -0.5
                def conv_half(hf):
                    ub = load_w(3088 + hf * 512, 512)
                    cbk = load_w(5136 + hf * 512, 512)
                    bbk = load_w(4112 + hf * 512, 512)
                    for cc in range(4):
                        for (t0, n) in TBS:
                            pu = fm(ub, cc, t0, n)
                            us = stg_f.next()
                            S.act([pu.t], [us.t]).copy(out=us.ap[:, 0:n], in_=pu.ap[:, 0:n])
                            pc = fm(cbk, cc, t0, n)
                            S.dve([pc.t, us.t], [t_z[cc]]).tensor_tensor(out=zT[:, cc, t0:t0 + n], in0=pc.ap[:, 0:n],
                                                                        in1=us.ap[:, 0:n], op=ALU.mult)
                            pbk = fm(bbk, cc, t0, n)
                            S.act([pbk.t], [t_bg[cc]]).copy(out=bgT[:, cc, t0:t0 + n], in_=pbk.ap[:, 0:n])
                    for cc in range(4):
                        ch = hf * 4 + cc
                        y = y_r.next()
                        z = zT[:, cc, :]
                        S.dve([t_z[cc], cw.t], [y.t]).tensor_scalar(out=y.ap, in0=z, scalar1=cw.ap[:, 1, ch:ch + 1],
                                                                    scalar2=None, op0=ALU.mult)

                        def acc(dst, src, tap):
                            S.dve([t_z[cc], cw.t, y.t], [y.t]).scalar_tensor_tensor(
                                out=dst, in0=src, scalar=cw.ap[:, tap, ch:ch + 1], in1=dst, op0=ALU.mult, op1=ALU.add)
                        acc(y.ap[:, 1:256], z[:, 0:255], 0)
                        acc(y.ap[:, 0:255], z[:, 1:256], 2)
                        if hf == 0:
                            yl = y.ap[:, 256:T].rearrange("p (r c) -> p r c", c=64)
                            zl = zT[:, cc, 256:T].rearrange("p (r c) -> p r c", c=64)
                            acc(yl[:, :, 1:64], zl[:, :, 0:63], 0)
                            acc(yl[:, :, 0:63], zl[:, :, 1:64], 2)
                        else:
                            acc(y.ap[:, 320:T], z[:, 256:T - 64], 0)
                            acc(y.ap[:, 256:T - 64], z[:, 320:T], 2)
                        ym = ym_r.next()
                        S.dve([y.t, t_bg[cc], cb.t], [ym.t]).scalar_tensor_tensor(
                            out=ym.ap, in0=y.ap, scalar=cb.ap[:, ch:ch + 1], in1=bgT[:, cc, :], op0=ALU.add, op1=ALU.mult)
                        S.dma("sp", out=mixT_d[8 + ch, :, :], in_=ym.ap, r=[ym.t])
                conv_half(0)
                wb = load_w(0, 512)
                for cc in range(4):
                    for (t0, n) in TBS:
                        b = fm(wb, cc, t0, n)
                        sg = stg_bf.next()
                        S.act([b.t], [sg.t]).copy(out=sg.ap[:, 0:n], in_=b.ap[:, 0:n])
                        S.dma("sp", out=qT_d[cc, :, t0:t0 + n], in_=sg.ap[:, 0:n], r=[sg.t])
                wb = load_w(512, 512)
                for cc in range(4):
                    for (t0, n) in TBS:
                        b = fm(wb, cc, t0, n)
                        sg = stg_bf.next()
                        S.act([b.t], [sg.t]).mul(out=sg.ap[:, 0:n], in_=b.ap[:, 0:n], mul=KS)
                        S.dma("sp", out=kT_d[cc, :, t0:t0 + n], in_=sg.ap[:, 0:n], r=[sg.t])
                for i in range(NT):
                    b = tm(wb, i, 512)
                    sg = stg_bf.next()
                    S.act([b.t], [sg.t]).mul(out=sg.ap, in_=b.ap, mul=KS)
                    S.dma("sp", out=k_d[i * 128:(i + 1) * 128, :], in_=sg.ap, r=[sg.t])
                for hf in range(2):
                    wb = load_w(1024 + hf * 512, 512)
                    for i in range(NT):
                        b = tm(wb, i, 512)
                        sg = stg_bf.next()
                        S.act([b.t], [sg.t]).copy(out=sg.ap, in_=b.ap)
                        S.dma("sp", out=v_d[i * 128:(i + 1) * 128, hf * 512:(hf + 1) * 512], in_=sg.ap, r=[sg.t])
                wb = load_w(2048, 16)
                for i in range(NT):
                    b = tm(wb, i, 16)
                    S.dve([b.t, bg_b.t], [gates_sb.t]).tensor_tensor(out=gates_sb.ap[:, i, :], in0=b.ap[:, 0:16],
                                                                   in1=bg_b.ap, op=ALU.add)
                S.dma("sp", out=gates_d.ap().rearrange("(i p) c -> p i c", p=P), in_=gates_sb.ap, r=[gates_sb.t])
                for hf in range(2):
                    wb = load_w(2064 + hf * 512, 512)
                    for i in range(NT):
                        b = tm(wb, i, 512)
                        sg = stg_bf.next()
                        S.act([b.t], [sg.t]).activation(out=sg.ap, in_=b.ap, func=AF.Sigmoid)
                        S.dma("sp", out=so_d[i * 128:(i + 1) * 128, hf * 512:(hf + 1) * 512], in_=sg.ap, r=[sg.t])
                conv_half(1)
                S.barrier()
            if stop_after == "M2":
                finish()
                return nc

            with ExitStack() as ph:
                gt = mk(ph, "gt", [P, NT, 16], F32)
                ex = mk(ph, "ex", [P, NT, 8], F32)
                spl = mk(ph, "spl", [P, NT, 8], F32)
                lfd = [mk(ph, "lfd%d" % d, [P, 72], F32) for d in range(2)]
                Bv = [mk(ph, "Bv%d" % d, [P, 72], F32) for d in range(2)]
                bsb = [mk(ph, "bsb%d" % d, [P, 72], F32) for d in range(2)]
                mxc = [mk(ph, "mxc%d" % d, [P, 1], F32) for d in range(2)]
                rows = [mk(ph, "rows%d" % d, [1, 4 * 72 + 4], F32) for d in range(2)]
                gb = [mk(ph, "gb%d" % d, [P, 72], F32) for d in range(2)]
                ev = [mk(ph, "ev%d" % d, [P, 72], F32) for d in range(2)]
                thr = [mk(ph, "thr%d" % d, [P, 72], F32) for d in range(2)]
                tmp72 = [mk(ph, "tmp72_%d" % d, [P, 72], F32) for d in range(2)]
                g_b = mk(ph, "g_b", [P, 1024], F32)
                S.dma("sp", out=gt.ap, in_=gates_d.ap().rearrange("(i p) c -> p i c", p=P), w=[gt.t])
                S.dma("sp", out=g_b.ap, in_=mhg[l:l + 1, :].partition_broadcast(P), w=[g_b.t])
                S.act([gt.t], [ex.t]).activation(out=ex.ap, in_=gt.ap[:, :, 8:16], func=AF.Exp, scale=-1.0)
                S.act([ex.t], [spl.t]).activation(out=spl.ap, in_=ex.ap, func=AF.Ln, bias=1.0)
                for d in range(2):
                    S.dve([spl.t], [lfd[d].t]).tensor_scalar(
                        out=lfd[d].ap.rearrange("p (c h) -> p c h", h=4), in0=spl.ap[:, :, d * 4:(d + 1) * 4],
                        scalar1=-1.0, scalar2=None, op0=ALU.mult)
                    pb_ = bank()
                    S.pe([mkf[d].t, lfd[d].t], [pb_.t]).matmul(pb_.ap[:, 0:72], mkf[d].ap, lfd[d].ap, start=True, stop=True)
                    S.dve([gt.t, pb_.t], [Bv[d].t]).tensor_tensor(
                        out=Bv[d].ap.rearrange("p (c h) -> p c h", h=4), in0=gt.ap[:, :, d * 4:(d + 1) * 4],
                        in1=pb_.ap[:, 0:72].rearrange("p (c h) -> p c h", h=4), op=ALU.subtract)
                    S.act([pb_.t], [bsb[d].t]).copy(out=bsb[d].ap, in_=pb_.ap[:, 0:72])
                    pe_ = bank()
                    S.pe([ones_col.t, lfd[d].t], [pe_.t]).matmul(pe_.ap[0:1, 0:72], ones_col.ap, lfd[d].ap,
                                                                 start=True, stop=True)
                    rw = rows[d]
                    S.act([pe_.t], [rw.t]).copy(out=rw.ap[0:1, 0:72], in_=pe_.ap[0:1, 0:72])
                    pt_ = bank()
                    S.pe([Bv[d].t, idf.t], [pt_.t]).transpose(out=pt_.ap[0:72, 0:128], in_=Bv[d].ap, identity=idf.ap)
                    S.dve([pt_.t], [mxc[d].t]).tensor_reduce(out=mxc[d].ap[0:72, 0:1], in_=pt_.ap[0:72, 0:128],
                                                             axis=AX.X, op=ALU.max)
                    pm_ = bank()
                    S.pe([mxc[d].t, idf.t], [pm_.t]).transpose(out=pm_.ap[0:1, 0:72], in_=mxc[d].ap[0:72, 0:1],
                                                               identity=idf.ap[0:72, 0:72])
                    S.act([pm_.t], [rw.t]).copy(out=rw.ap[0:1, 72:144], in_=pm_.ap[0:1, 0:72])
                    mst = rw.ap[0:1, 288:292]
                    S.dve([], [rw.t]).memset(mst, 0.0)
                    for c in ORDER[d]:
                        bend_c = rw.ap[0:1, c * 4:c * 4 + 4]
                        mx_c = rw.ap[0:1, 72 + c * 4:72 + c * 4 + 4]
                        M_c = rw.ap[0:1, 144 + c * 4:144 + c * 4 + 4]
                        g_c = rw.ap[0:1, 216 + c * 4:216 + c * 4 + 4]
                        S.dve([rw.t], [rw.t]).tensor_tensor(out=M_c, in0=mst, in1=mx_c, op=ALU.max)
                        S.dve([rw.t], [rw.t]).tensor_tensor(out=g_c, in0=mst, in1=M_c, op=ALU.subtract)
                        S.dve([rw.t], [rw.t]).tensor_tensor(out=mst, in0=bend_c, in1=M_c, op=ALU.add)
                    S.act([rw.t], [rw.t]).activation(out=rw.ap[0:1, 216:288], in_=rw.ap[0:1, 216:288], func=AF.Exp)
                    pM = bank()
                    S.pe([ones_row.t, rw.t], [pM.t]).matmul(pM.ap[:, 0:72], ones_row.ap, rw.ap[0:1, 144:216],
                                                            start=True, stop=True)
                    pG = bank()
                    S.pe([ones_row.t, rw.t], [pG.t]).matmul(pG.ap[:, 0:72], ones_row.ap, rw.ap[0:1, 216:288],
                                                            start=True, stop=True)
                    S.act([pG.t], [gb[d].t]).copy(out=gb[d].ap, in_=pG.ap[:, 0:72])
                    S.dve([Bv[d].t, pM.t], [tmp72[d].t]).tensor_tensor(out=tmp72[d].ap, in0=Bv[d].ap, in1=pM.ap[:, 0:72],
                                                                       op=ALU.subtract)
                    S.act([tmp72[d].t], [ev[d].t]).activation(out=ev[d].ap, in_=tmp72[d].ap, func=AF.Exp)
                    S.dve([bsb[d].t, pM.t], [tmp72[d].t]).tensor_tensor(out=tmp72[d].ap, in0=bsb[d].ap, in1=pM.ap[:, 0:72],
                                                                        op=ALU.add)
                    S.act([tmp72[d].t], [thr[d].t]).activation(out=thr[d].ap, in_=tmp72[d].ap, func=AF.Exp, scale=-1.0)

                NHB = 2
                qTh = [mk(ph, "qTh%d" % i, [P, T], BF16) for i in range(NHB)]
                kTh = [mk(ph, "kTh%d" % i, [P, T], BF16) for i in range(NHB)]
                kh = [mk(ph, "kh%d" % i, [P, NT, 128], BF16) for i in range(NHB)]
                v1h = [mk(ph, "v1h%d" % i, [P, NT, 257], BF16) for i in range(NHB)]
                soh = [mk(ph, "soh%d" % i, [P, NT, 256], BF16) for i in range(NHB)]
                mixh = [mk(ph, "mixh%d" % i, [P, 2, T], BF16) for i in range(NHB)]
                A_ = [[mk(ph, "A%d_%d" % (i, d), [P, 257], F32) for d in range(2)] for i in range(NHB)]
                Ab = [[mk(ph, "Ab%d_%d" % (i, d), [P, 257], BF16) for d in range(2)] for i in range(NHB)]
                raw = [[sbuf(ph, "raw%d_%d" % (i, d), [P, NT, 257], F32) for d in range(2)] for i in range(NHB)]
                t_raw = [[[Tile("raw%d_%d_%d" % (i, d, c)) for c in range(NT)] for d in range(2)] for i in range(NHB)]
                rin = [[mk(ph, "rin%d_%d" % (i, d), [P, 3 * NT], F32) for d in range(2)] for i in range(NHB)]
                sw_r = ring(ph, "sw_r", [P, P], BF16, 6)
                ke_r = ring(ph, "ke_r", [P, P], BF16, 6)
                hs_r = ring(ph, "hs_r", [P, 256], F32, 3)
                gs_r = ring(ph, "gs_r", [P, 256], F32, 3)
                mt_r = ring(ph, "mt_r", [P, 256], BF16, 3)
                junk = mk(ph, "junk", [P, 256], F32)
                ss = mk(ph, "ss", [P, 3 * NT], F32)
                for grp in range(2):
                    for hh in range(NHB):
                        h = grp * NHB + hh
                        S.dma("sp", out=qTh[hh].ap, in_=qT_d[h, :, :], w=[qTh[hh].t])
                        S.dma("sp", out=kTh[hh].ap, in_=kT_d[h, :, :], w=[kTh[hh].t])
                        S.dma("sp", out=kh[hh].ap, in_=k_d[:, h * 128:(h + 1) * 128].rearrange("(c p) k -> p c k", p=P),
                              w=[kh[hh].t])
                        S.dve([], [v1h[hh].t]).memset(v1h[hh].ap[:, :, 256:257], 1.0)
                        S.dma("sp", out=v1h[hh].ap[:, :, 0:256],
                              in_=v_d[:, h * 256:(h + 1) * 256].rearrange("(c p) v -> p c v", p=P), w=[v1h[hh].t])
                        S.dma("sp", out=soh[hh].ap, in_=so_d[:, h * 256:(h + 1) * 256].rearrange("(c p) v -> p c v", p=P),
                              w=[soh[hh].t])
                    for s_ in range(NT):
                        for hh in range(NHB):
                            h = grp * NHB + hh
                            for d in range(2):
                                c = ORDER[d][s_]
                                col = c * 4 + h
                                nxt = (ORDER[d][s_ + 1] * 4 + h) if s_ < NT - 1 else None
                                tsl = slice(c * 128, (c + 1) * 128)
                                A, Abf = A_[hh][d], Ab[hh][d]
                                pS = bank()
                                S.pe([kTh[hh].t, qTh[hh].t], [pS.t]).matmul(pS.ap[:, 0:128], kTh[hh].ap[:, tsl],
                                                                          qTh[hh].ap[:, tsl], start=True, stop=True)
                                sw = sw_r.next()
                                S.dve([pS.t, ev[d].t, mkf[d].t], [sw.t]).scalar_tensor_tensor(
                                    out=sw.ap, in0=pS.ap[:, 0:128], scalar=ev[d].ap[:, col:col + 1], in1=mkf[d].ap,
                                    op0=ALU.mult, op1=ALU.mult)
                                pN = bank()
                                if s_ == 0:
                                    S.pe([sw.t, v1h[hh].t], [pN.t]).matmul(pN.ap[:, 0:257], sw.ap, v1h[hh].ap[:, c, :],
                                                                          start=True, stop=True)
                                else:
                                    S.pe([sw.t, v1h[hh].t], [pN.t], sig=False).matmul(
                                        pN.ap[:, 0:257], sw.ap, v1h[hh].ap[:, c, :], start=True, stop=False)
                                    S.pe([qTh[hh].t, Abf.t], [pN.t]).matmul(pN.ap[:, 0:257], qTh[hh].ap[:, tsl], Abf.ap,
                                                                           start=False, stop=True)
                                S.act([pN.t], [t_raw[hh][d][c]]).copy(out=raw[hh][d][:, c, :], in_=pN.ap[:, 0:257])
                                if nxt is not None:
                                    ke = ke_r.next()
                                    S.pool([kh[hh].t, ev[d].t, gb[d].t], [ke.t]).tensor_scalar(
                                        out=ke.ap, in0=kh[hh].ap[:, c, :], scalar1=ev[d].ap[:, col:col + 1],
                                        scalar2=gb[d].ap[:, nxt:nxt + 1], op0=ALU.mult, op1=ALU.mult)
                                    pC = bank()
                                    S.pe([ke.t, v1h[hh].t], [pC.t]).matmul(pC.ap[:, 0:257], ke.ap, v1h[hh].ap[:, c, :],
                                                                          start=True, stop=True)
                                    if s_ == 0:
                                        S.dve([pC.t], [A.t]).tensor_copy(out=A.ap, in_=pC.ap[:, 0:257])
                                    else:
                                        S.dve([A.t, gb[d].t, pC.t], [A.t]).scalar_tensor_tensor(
                                            out=A.ap, in0=A.ap, scalar=gb[d].ap[:, nxt:nxt + 1], in1=pC.ap[:, 0:257],
                                            op0=ALU.mult, op1=ALU.add)
                                    S.act([A.t], [Abf.t]).copy(out=Abf.ap, in_=A.ap)
                    for hh in range(NHB):
                        h = grp * NHB + hh
                        for d in range(2):
                            rn = rin[hh][d]
                            den = raw[hh][d][:, :, 256]
                            thr_h = thr[d].ap.rearrange("p (c h) -> p c h", h=4)[:, :, h]
                            rd = t_raw[hh][d]
                            S.dve(rd, [rn.t]).tensor_scalar(out=rn.ap[:, 0:NT], in0=den, scalar1=-1.0, scalar2=None,
                                                            op0=ALU.mult)
                            S.dve(rd + [thr[d].t], [rn.t]).tensor_tensor(out=rn.ap[:, NT:2 * NT], in0=den, in1=thr_h,
                                                                         op=ALU.max)
                            S.dve([rn.t], [rn.t]).tensor_tensor(out=rn.ap[:, NT:2 * NT], in0=rn.ap[:, NT:2 * NT],
                                                                in1=rn.ap[:, 0:NT], op=ALU.max)
                            S.dve([rn.t], [rn.t]).tensor_scalar(out=rn.ap[:, NT:2 * NT], in0=rn.ap[:, NT:2 * NT],
                                                                scalar1=1e-30, scalar2=None, op0=ALU.max)
                            S.dve([rn.t], [rn.t]).reciprocal(rn.ap[:, 2 * NT:3 * NT], rn.ap[:, NT:2 * NT])
                        mh = mixh[hh]
                        for c in range(NT):
                            hs = hs_r.next()
                            S.dve([t_raw[hh][0][c], rin[hh][0].t], [hs.t]).tensor_scalar(
                                out=hs.ap, in0=raw[hh][0][:, c, 0:256], scalar1=rin[hh][0].ap[:, 2 * NT + c:2 * NT + c + 1],
                                scalar2=None, op0=ALU.mult)
                            S.dve([t_raw[hh][1][c], rin[hh][1].t, hs.t], [hs.t]).scalar_tensor_tensor(
                                out=hs.ap, in0=raw[hh][1][:, c, 0:256], scalar=rin[hh][1].ap[:, 2 * NT + c:2 * NT + c + 1],
                                in1=hs.ap, op0=ALU.mult, op1=ALU.add)
                            S.act([hs.t], [junk.t, ss.t]).activation(out=junk.ap, in_=hs.ap, func=AF.Square,
                                                                     accum_out=ss.ap[:, c:c + 1])
                            S.act([ss.t], [ss.t]).activation(out=ss.ap[:, NT + c:NT + c + 1], in_=ss.ap[:, c:c + 1],
                                                             func=AF.Sqrt, scale=1.0 / 256.0, bias=EPS)
                            S.dve([ss.t], [ss.t]).reciprocal(ss.ap[:, 2 * NT + c:2 * NT + c + 1], ss.ap[:, NT + c:NT + c + 1])
                            gs = gs_r.next()
                            S.pool([soh[hh].t, g_b.t], [gs.t]).tensor_tensor(out=gs.ap, in0=soh[hh].ap[:, c, :],
                                                                            in1=g_b.ap[:, h * 256:(h + 1) * 256], op=ALU.mult)
                            mt = mt_r.next()
                            S.dve([hs.t, ss.t, gs.t], [mt.t]).scalar_tensor_tensor(
                                out=mt.ap, in0=hs.ap, scalar=ss.ap[:, 2 * NT + c:2 * NT + c + 1], in1=gs.ap,
                                op0=ALU.mult, op1=ALU.mult)
                            pT = bank()
                            pbT = pT.ap.bitcast(BF16)
                            for vv in range(2):
                                S.pe([mt.t, idb.t], [pT.t], sig=(vv == 1)).transpose(
                                    out=pbT[:, vv * 128:(vv + 1) * 128], in_=mt.ap[:, vv * 128:(vv + 1) * 128],
                                    identity=idb.ap)
                            for vv in range(2):
                                S.act([pT.t], [mh.t]).copy(out=mh.ap[:, vv, c * 128:(c + 1) * 128],
                                                           in_=pbT[:, vv * 128:(vv + 1) * 128])
                        for vv in range(2):
                            S.dma("sp", out=mixT_d[h * 2 + vv, :, :], in_=mh.ap[:, vv, :], r=[mh.t])
                S.barrier()
            if stop_after == "M3":
                finish()
                return nc

            with ExitStack() as ph:
                wo = sbuf(ph, "wo", [P, KC, D], BF16)
                t_wo = [Tile("wo%d" % i) for i in range(4)]
                g1b = [mk(ph, "g1b%d" % r, [P, D], F32) for r in range(2)]
                lg = mk(ph, "lg", [P, D], F32)
                lb = mk(ph, "lb", [P, D], F32)
                x_r = ring(ph, "x6_r", [P, D], F32, 3)
                tmp_r = ring(ph, "tmp_r", [P, D], F32, 2)
                xn_r = ring(ph, "xn6_r", [P, D], BF16, 2)
                mix_r = ring(ph, "mix_r", [P, KC, 128], BF16, 2)
                h2_r = ring(ph, "h2_r", [P, KC, 128], BF16, 2)
                xnf_r = ring(ph, "xnf_r", [P, D], F32, 2)
                h2f_r = ring(ph, "h2f_r", [P, KC, 128], F32, 2)
                wrf = mk(ph, "wrf", [P, KC, 16], F32)
                lg_sb = mk(ph, "lg_sb", [P, NT, 16], F32)
                S.dma("sp", out=wrf.ap, in_=w_router[:, :, :], w=[wrf.t])
                for cbk in range(4):
                    S.dma("pool", out=wo[:, :, cbk * 512:(cbk + 1) * 512],
                          in_=w_out[l, :, cbk * 512:(cbk + 1) * 512].rearrange("(kc p) n -> p kc n", p=P), w=[t_wo[cbk]])
                for r in range(2):
                    gate_bcast(g1b[r], l, 2, r)
                S.dma("sp", out=lg.ap, in_=ln1_g[l:l + 1, :].partition_broadcast(P), w=[lg.t])
                S.dma("sp", out=lb.ap, in_=ln1_b[l:l + 1, :].partition_broadcast(P), w=[lb.t])
                for i in range(NT):
                    r = 1 if i < 2 else 0
                    rs_ = slice(i * 128, (i + 1) * 128)
                    mx = mix_r.next()
                    S.dma("sp", out=mx.ap, in_=mixT_d[:, :, rs_].rearrange("c p t -> p c t"), w=[mx.t])
                    xt = x_r.next()
                    S.dma("sp", out=xt.ap, in_=xsrc[rs_, :], w=[xt.t])
                    bks = [bank() for _ in range(4)]
                    for cbk in range(4):
                        for kc in range(KC):
                            S.pe([mx.t, t_wo[cbk]], [bks[cbk].t], sig=(kc == KC - 1)).matmul(
                                bks[cbk].ap, mx.ap[:, kc, :], wo[:, kc, cbk * 512:(cbk + 1) * 512],
                                start=(kc == 0), stop=(kc == KC - 1))
                    tmp = tmp_r.next()
                    for cbk in range(4):
                        cs_ = slice(cbk * 512, (cbk + 1) * 512)
                        S.dve([bks[cbk].t, g1b[r].t], [tmp.t]).tensor_tensor(out=tmp.ap[:, cs_], in0=bks[cbk].ap,
                                                                            in1=g1b[r].ap[:, cs_], op=ALU.mult)
                    S.dve([xt.t, tmp.t], [xt.t]).scalar_tensor_tensor(out=xt.ap, in0=xt.ap, scalar=ALPHA, in1=tmp.ap,
                                                                      op0=ALU.mult, op1=ALU.add)
                    ln_affine(xt, lg, lb)
                    S.dma("sp", out=xl_d[rs_, :], in_=xt.ap, r=[xt.t])
                    h2 = h2_r.next()
                    mean2, rstd2, ts2 = ln_stats(xt.ap, xt.t)
                    xnf = xnf_r.next()
                    S.dve([xt.t, ts2], [xnf.t]).tensor_scalar(out=xnf.ap, in0=xt.ap, scalar1=mean2, scalar2=rstd2,
                                                              op0=ALU.subtract, op1=ALU.mult)
                    xn = xn_r.next()
                    S.pool([xnf.t], [xn.t]).tensor_copy(out=xn.ap, in_=xnf.ap)
                    for half in range(2):
                        b = bank()
                        pb = b.ap.bitcast(BF16)
                        for cc in range(8):
                            c = half * 8 + cc
                            S.pe([xn.t, idb.t], [b.t], sig=(cc == 7)).transpose(
                                out=pb[:, cc * 128:(cc + 1) * 128], in_=xn.ap[:, c * 128:(c + 1) * 128], identity=idb.ap)
                        for cc in range(8):
                            c = half * 8 + cc
                            S.act([b.t, modT.t], [h2.t]).activation(
                                out=h2.ap[:, c, :], in_=pb[:, cc * 128:(cc + 1) * 128], func=AF.Identity,
                                scale=modT.ap[:, l, 3, c, r:r + 1], bias=modT.ap[:, l, 2, c, r:r + 1])
                    S.dma("sp", out=h2T_d[:, :, rs_], in_=h2.ap, r=[h2.t])
                    h2f = h2f_r.next()
                    for qd in range(4):
                        b = bank()
                        for cc in range(4):
                            c = qd * 4 + cc
                            S.pe([xnf.t, idf.t], [b.t], sig=(cc == 3)).transpose(
                                out=b.ap[:, cc * 128:(cc + 1) * 128], in_=xnf.ap[:, c * 128:(c + 1) * 128], identity=idf.ap)
                        for cc in range(4):
                            c = qd * 4 + cc
                            S.act([b.t, modT.t], [h2f.t]).activation(
                                out=h2f.ap[:, c, :], in_=b.ap[:, cc * 128:(cc + 1) * 128], func=AF.Identity,
                                scale=modT.ap[:, l, 3, c, r:r + 1], bias=modT.ap[:, l, 2, c, r:r + 1])
                    pl = bank()
                    for kc in range(KC):
                        S.pe([h2f.t, wrf.t], [pl.t], sig=(kc == KC - 1)).matmul(
                            pl.ap[:, 0:16], h2f.ap[:, kc, :], wrf.ap[:, kc, :], start=(kc == 0), stop=(kc == KC - 1))
                    S.act([pl.t], [lg_sb.t]).copy(out=lg_sb.ap[:, i, :], in_=pl.ap[:, 0:16])
                S.dma("sp", out=logit_d.ap().rearrange("(i p) e -> p i e", p=P), in_=lg_sb.ap, r=[lg_sb.t])
                S.barrier()
            if stop_after == "M6":
                finish()
                return nc

            with ExitStack() as ph:
                h2s = sbuf(ph, "h2s", [P, KC, TH], BF16)
                t_h2s = [Tile("h2s%d" % i) for i in range(KC)]
                acc = sbuf(ph, "acc", [P, NTH, D], F32)
                t_acc = [Tile("acc%d" % i) for i in range(NTH)]
                gates = mk(ph, "gates", [P, NTH, 16], F32)
                lgt = mk(ph, "lgt", [P, 2, NTH, 16], F32)
                lgs = mk(ph, "lgs", [P, NTH, 16], F32)
                brb = mk(ph, "brb", [P, 16], F32)
                rt_r = ring(ph, "rt_r", [P, 96], F32, 2)
                with ExitStack() as ph2:
                    ld_r = ring(ph2, "ld_r", [P, 2, TH], BF16, 2)
                    tb_r = ring(ph2, "tb_r", [P, TH], F32, 2)
                    for kc in range(KC):
                        a = ld_r.next()
                        S.dma("sp", out=a.ap, in_=h2T_d[:, kc, :].rearrange("p (j t) -> p j t", j=2), w=[a.t])
                        tb_ = tb_r.next()
                        S.dve([a.t, selt.t], [tb_.t]).tensor_scalar(out=tb_.ap, in0=a.ap[:, 0, :], scalar1=selt.ap[:, 0:1],
                                                                    scalar2=None, op0=ALU.mult)
                        S.dve([a.t, selt.t, tb_.t], [t_h2s[kc]]).scalar_tensor_tensor(
                            out=h2s[:, kc, :], in0=a.ap[:, 1, :], scalar=selt.ap[:, 1:2], in1=tb_.ap,
                            op0=ALU.mult, op1=ALU.add)
                    S.dma("sp", out=lgt.ap, in_=logit_d.ap().rearrange("(j i p) e -> p j i e", j=2, p=P), w=[lgt.t])
                    S.dve([lgt.t, selt.t], [lgs.t]).tensor_scalar(out=lgs.ap, in0=lgt.ap[:, 0, :, :], scalar1=selt.ap[:, 0:1],
                                                                  scalar2=None, op0=ALU.mult)
                    S.dve([lgt.t, selt.t, lgs.t], [lgs.t]).scalar_tensor_tensor(
                        out=lgs.ap, in0=lgt.ap[:, 1, :, :], scalar=selt.ap[:, 1:2], in1=lgs.ap, op0=ALU.mult, op1=ALU.add)
                    S.dma("sp", out=brb.ap, in_=b_router[0:1, :].partition_broadcast(P), w=[brb.t])
                    for i in range(NTH):
                        S.pool([], [t_acc[i]]).memset(acc[:, i, :], 0.0)
                    for i in range(NTH):
                        rt = rt_r.next()
                        a = rt.ap
                        s_ = a[:, 0:16]
                        sb_ = a[:, 16:32]
                        sb2 = a[:, 32:48]
                        m1 = a[:, 48:52]
                        m2 = a[:, 52:56]
                        gsc = a[:, 56:60]
                        gmx = a[:, 60:61]
                        geq = a[:, 61:65]
                        selm = a[:, 65:81]
                        den = a[:, 81:82]
                        rden = a[:, 82:83]
                        S.act([lgs.t], [rt.t]).activation(out=s_, in_=lgs.ap[:, i, :], func=AF.Sigmoid)
                        S.dve([rt.t, brb.t], [rt.t]).tensor_tensor(out=sb_, in0=s_, in1=brb.ap, op=ALU.add)
                        S.dve([rt.t], [rt.t]).tensor_reduce(out=m1, in_=sb_.rearrange("p (g e) -> p g e", e=4),
                                                            axis=AX.X, op=ALU.max)
                        for g in range(4):
                            gsl = slice(g * 4, g * 4 + 4)
                            S.dve([rt.t], [rt.t]).tensor_scalar(out=sb2[:, gsl], in0=sb_[:, gsl], scalar1=m1[:, g:g + 1],
                                                                scalar2=-1e9, op0=ALU.is_equal, op1=ALU.mult)
                        S.dve([rt.t], [rt.t]).tensor_tensor(out=sb2, in0=sb2, in1=sb_, op=ALU.add)
                        S.dve([rt.t], [rt.t]).tensor_reduce(out=m2, in_=sb2.rearrange("p (g e) -> p g e", e=4),
                                                            axis=AX.X, op=ALU.max)
                        S.dve([rt.t], [rt.t]).tensor_tensor(out=gsc, in0=m1, in1=m2, op=ALU.add)
                        S.dve([rt.t], [rt.t]).tensor_reduce(out=gmx, in_=gsc, axis=AX.X, op=ALU.max)
                        S.dve([rt.t], [rt.t]).tensor_scalar(out=geq, in0=gsc, scalar1=gmx, scalar2=None, op0=ALU.is_equal)
                        for g in range(4):
                            gsl = slice(g * 4, g * 4 + 4)
                            S.dve([rt.t], [rt.t]).tensor_scalar(out=selm[:, gsl], in0=sb_[:, gsl], scalar1=m2[:, g:g + 1],
                                                                scalar2=geq[:, g:g + 1], op0=ALU.is_ge, op1=ALU.mult)
                        S.dve([rt.t], [rt.t]).tensor_tensor(out=selm, in0=selm, in1=s_, op=ALU.mult)
                        S.dve([rt.t], [rt.t]).tensor_reduce(out=den, in_=selm, axis=AX.X, op=ALU.add)
                        S.dve([rt.t], [rt.t]).reciprocal(rden, den)
                        S.dve([rt.t], [gates.t]).tensor_scalar(out=gates.ap[:, i, :], in0=selm, scalar1=rden, scalar2=None,
                                                               op0=ALU.mult)
                    S.barrier()
                if debug:
                    S.dma("sp", out=dbg_gates[:, :], in_=gates.ap.rearrange("p i e -> p (i e)"), r=[gates.t])
                aT = sbuf(ph, "aT", [P, 8, TH], BF16)
                t_aT = [Tile("aT%d" % i) for i in range(8)]
                er = ring(ph, "er", [P, KC, 512], BF16, 4)
                sl_r = ring(ph, "sl_r", [P, 512], BF16, 3)
                TBM = [(0, 512), (512, 512), (1024, 128)]
                for e in range(NE):
                    for fh in range(2):
                        s1 = er.next()
                        S.dma("pool", out=s1.ap, in_=w1[l, e, :, fh * 512:(fh + 1) * 512].rearrange("(kc p) n -> p kc n", p=P),
                              w=[s1.t])
                        s3 = er.next()
                        S.dma("pool", out=s3.ap, in_=w3[l, e, :, fh * 512:(fh + 1) * 512].rearrange("(kc p) n -> p kc n", p=P),
                              w=[s3.t])
                        for fcl in range(4):
                            fc = fh * 4 + fcl
                            for (t0, n) in TBM:
                                p1 = bank()
                                for kc in range(KC):
                                    S.pe([s1.t, t_h2s[kc]], [p1.t], sig=(kc == KC - 1)).matmul(
                                        p1.ap[:, 0:n], s1.ap[:, kc, fcl * 128:(fcl + 1) * 128], h2s[:, kc, t0:t0 + n],
                                        start=(kc == 0), stop=(kc == KC - 1))
                                p3 = bank()
                                for kc in range(KC):
                                    S.pe([s3.t, t_h2s[kc]], [p3.t], sig=(kc == KC - 1)).matmul(
                                        p3.ap[:, 0:n], s3.ap[:, kc, fcl * 128:(fcl + 1) * 128], h2s[:, kc, t0:t0 + n],
                                        start=(kc == 0), stop=(kc == KC - 1))
                                sl = sl_r.next()
                                S.act([p1.t], [sl.t]).activation(out=sl.ap[:, 0:n], in_=p1.ap[:, 0:n], func=AF.Silu)
                                S.dve([p3.t, sl.t], [t_aT[fc]]).tensor_tensor(out=aT[:, fc, t0:t0 + n], in0=p3.ap[:, 0:n],
                                                                             in1=sl.ap[:, 0:n], op=ALU.mult)
                    for dh in range(2):
                        s2 = er.next()
                        s2v = s2.ap.rearrange("p a b -> p (a b)").rearrange("p (f n) -> p f n", n=1024)
                        S.dma("pool", out=s2v, in_=w2[l, e, :, dh * 1024:(dh + 1) * 1024].rearrange("(f p) n -> p f n", p=P),
                              w=[s2.t])
                        for i in range(NTH):
                            for cbk in range(2):
                                py = bank()
                                for fc in range(8):
                                    S.pe([t_aT[fc], s2.t], [py.t], sig=(fc == 7)).matmul(
                                        py.ap, aT[:, fc, i * 128:(i + 1) * 128], s2v[:, fc, cbk * 512:(cbk + 1) * 512],
                                        start=(fc == 0), stop=(fc == 7))
                                c0 = dh * 1024 + cbk * 512
                                S.dve([py.t, gates.t, t_acc[i]], [t_acc[i]]).scalar_tensor_tensor(
                                    out=acc[:, i, c0:c0 + 512], in0=py.ap, scalar=gates.ap[:, i, e:e + 1],
                                    in1=acc[:, i, c0:c0 + 512], op0=ALU.mult, op1=ALU.add)
                for i in range(NTH):
                    S.dma("sp", out=cc_in[i][:, :], in_=acc[:, i, :], r=[t_acc[i]])
                    if debug:
                        S.dma("sp", out=dbg_moe[i * 128:(i + 1) * 128, :], in_=acc[:, i, :], r=[t_acc[i]])
                if stop_after == "M8pre":
                    finish()
                    return nc
                S._pre("pool", [], t_acc)
                for i in range(NTH):
                    cc_n[0] += 1
                    nc.gpsimd.collective_compute("AllGather", ALU.bypass, replica_groups=RG,
                                                 ins=[cc_in[i].ap().opt()], outs=[cc_out[i].ap().opt()]).then_inc(cc_sem, 1)
                S.extra = [("cc", cc_sem, cc_n[0])]
                S.barrier()
            if stop_after == "M8":
                finish()
                return nc

            with ExitStack() as ph:
                g2b = [mk(ph, "g2b%d" % r, [P, D], F32) for r in range(2)]
                lg = mk(ph, "lg2", [P, D], F32)
                lb = mk(ph, "lb2", [P, D], F32)
                x_r = ring(ph, "x9_r", [P, D], F32, 3)
                mo_r = ring(ph, "mo_r", [P, D], F32, 3)
                for r in range(2):
                    gate_bcast(g2b[r], l, 5, r)
                S.dma("sp", out=lg.ap, in_=ln2_g[l:l + 1, :].partition_broadcast(P), w=[lg.t])
                S.dma("sp", out=lb.ap, in_=ln2_b[l:l + 1, :].partition_broadcast(P), w=[lb.t])
                for i in range(NT):
                    r = 1 if i < 2 else 0
                    rs_ = slice(i * 128, (i + 1) * 128)
                    xt = x_r.next()
                    S.dma("sp", out=xt.ap, in_=xl_d[rs_, :], w=[xt.t])
                    mo = mo_r.next()
                    S.dma("sp", out=mo.ap, in_=(cc_out[i][0:P, :] if i < NTH else cc_out[i - NTH][P:2 * P, :]), w=[mo.t])
                    S.dve([mo.t, g2b[r].t], [mo.t]).tensor_tensor(out=mo.ap, in0=mo.ap, in1=g2b[r].ap, op=ALU.mult)
                    S.dve([xt.t, mo.t], [xt.t]).scalar_tensor_tensor(out=xt.ap, in0=xt.ap, scalar=ALPHA, in1=mo.ap,
                                                                     op0=ALU.mult, op1=ALU.add)
                    ln_affine(xt, lg, lb)
                    S.dma("sp", out=xl_d[rs_, :], in_=xt.ap, r=[xt.t])
                    if last and i >= 2:
                        S.dma("sp", out=out[(i - 2) * 128:(i - 1) * 128, :], in_=xt.ap, r=[xt.t])
                S.barrier()
        finish()
    return nc


def make_inputs(inp, depth=4, cores=range(8)):
    f = np.float32
    x = np.asarray(inp["x"], f)
    c = np.asarray(inp["c"], f)
    ctx = np.asarray(inp["ctx"], f)
    c_ctx = np.asarray(inp["c_ctx"], f)
    b_ada = np.ascontiguousarray(np.asarray(inp["b_ada"], f)[:depth])
    conv_w = np.asarray(inp["conv_w"], f)[:depth]
    conv_b = np.asarray(inp["conv_b"], f)[:depth]
    ncols = depth * 1536
    w_ada_flat = np.asarray(inp["w_ada"], f)[:depth].transpose(1, 0, 2).reshape(2048, depth * 12288)
    b_ada_flat = b_ada.reshape(depth * 12288)
    cond5 = np.concatenate([c[:4], c_ctx[None, :]], axis=0)
    if cond5.shape[0] < 5:
        cond5 = np.concatenate([np.repeat(c[:1], 4, 0), c_ctx[None, :]], axis=0)
    cond5 = np.ascontiguousarray(cond5.T.reshape(16, 128, 5).transpose(1, 0, 2))
    common = {
        "cond5": cond5,
        "w_in": np.ascontiguousarray(np.asarray(inp["w_in"], f)[:depth]),
        "bgate": np.ascontiguousarray(np.concatenate([np.asarray(inp["b_igate"], f)[:depth],
                                                      np.asarray(inp["b_fgate"], f)[:depth]], axis=-1)),
        "mhg": np.ascontiguousarray(np.asarray(inp["mh_norm_g"], f)[:depth]),
        "conv_w": np.ascontiguousarray(conv_w.reshape(depth, 3, 8, 128).transpose(0, 3, 1, 2)),
        "conv_b": np.ascontiguousarray(conv_b.reshape(depth, 8, 128).transpose(0, 2, 1)),
        "w_out": np.ascontiguousarray(np.asarray(inp["w_out"], f)[:depth]),
        "ln1_g": np.ascontiguousarray(np.asarray(inp["ln1_g"], f)[:depth]),
        "ln1_b": np.ascontiguousarray(np.asarray(inp["ln1_b"], f)[:depth]),
        "ln2_g": np.ascontiguousarray(np.asarray(inp["ln2_g"], f)[:depth]),
        "ln2_b": np.ascontiguousarray(np.asarray(inp["ln2_b"], f)[:depth]),
        "w_router": np.ascontiguousarray(np.asarray(inp["w_router"], f).reshape(16, 128, 16).transpose(1, 0, 2)),
        "b_router": np.ascontiguousarray(np.asarray(inp["b_router"], f).reshape(1, 16)),
        "w1": np.ascontiguousarray(np.asarray(inp["w1"], f)[:depth]),
        "w3": np.ascontiguousarray(np.asarray(inp["w3"], f)[:depth]),
        "w2": np.ascontiguousarray(np.asarray(inp["w2"], f)[:depth]),
        "ident": np.eye(128, dtype=f),
        "masks": np.stack([np.triu(np.ones((128, 128), f)), np.tril(np.ones((128, 128), f))]),
    }
    in_maps = []
    for r in cores:
        b, j = r // 2, r % 2
        m = dict(common)
        m["x_in"] = np.ascontiguousarray(np.concatenate([ctx[b], x[b]], axis=0))
        m["w_ada_sh"] = np.ascontiguousarray(w_ada_flat[:, r * ncols:(r + 1) * ncols])
        m["b_ada_sh"] = np.ascontiguousarray(b_ada_flat[r * ncols:(r + 1) * ncols].reshape(depth * 12, 128).T)
        sb_ = np.zeros((128, 4), f)
        sb_[:, b] = 1.0
        m["selb"] = sb_
        s = np.zeros((128, 2), f)
        s[:, j] = 1.0
        m["sel"] = s
        in_maps.append(m)
    return in_maps


def kernel(**inputs):
    nc = build(depth=4)
    in_maps = make_inputs(inputs, depth=4)
    res = run_bass_kernel_spmd(nc, in_maps, core_ids=list(range(8)))
    outs = [np.asarray(res.results[2 * b]["out"], np.float32) for b in range(4)]
    return np.stack(outs, axis=0)
```

```python
import numpy as np
from contextlib import ExitStack
import concourse.bass as bass
import concourse.mybir as mybir
from concourse.bass_utils import run_bass_kernel_spmd

F32 = mybir.dt.float32
BF16 = mybir.dt.bfloat16
AF = mybir.ActivationFunctionType
ALU = mybir.AluOpType
AX = mybir.AxisListType

P = 128
D = 2048
KC = 16
T = 2304
NT = 18
TH = 1152
NTH = 9
NE = 16
FF = 1024
DIN = 6160
ALPHA = 8.0 ** 0.25
EPS = 1e-6
RG = [[0, 1], [2, 3], [4, 5], [6, 7]]
ORDER = [list(range(18)), [1, 0] + list(range(17, 1, -1))]
SAME_ENGINE_SYNC = True


class Tile:
    __slots__ = ("name", "w", "r", "ds", "excl")

    def __init__(self, name, excl=False):
        self.name = name
        self.w = None
        self.r = {}
        self.ds = None
        self.excl = excl


class _DS:
    def __init__(self, name, sem):
        self.name = name
        self.sem = sem
        self.cnt = 0


class _Px:
    def __init__(self, s, e, r, w, sig):
        self.s, self.e, self.r, self.w, self.sig = s, e, r, w, sig

    def __getattr__(self, name):
        def f(*a, **k):
            self.s._pre(self.e, self.r, self.w)
            ins = getattr(self.s.eng[self.e], name)(*a, **k)
            self.s._post(self.e, ins, self.r, self.w, self.sig)
            return ins
        return f


class Sched:
    def __init__(self, nc, st, n_dsem=56):
        self.nc = nc
        self.eng = {"pe": nc.tensor, "act": nc.scalar, "dve": nc.vector, "pool": nc.gpsimd, "sp": nc.sync}
        self.sem = {k: st.enter_context(nc.semaphore("s_" + k)) for k in self.eng}
        self.cnt = {k: 0 for k in self.eng}
        self.seen = {k: {} for k in self.eng}
        self.dsp = [_DS("d%d" % i, st.enter_context(nc.semaphore("d%d" % i))) for i in range(n_dsem)]
        self.dsi = 0
        self.extra = []

    def _pre(self, e, r, w):
        deps = {}

        def add(d):
            if d is None:
                return
            k, sem, v = d
            if k not in deps or deps[k][1] < v:
                deps[k] = (sem, v)
        for t in r:
            add(t.w)
        for t in w:
            add(t.w)
            for d in t.r.values():
                add(d)
        eng = self.eng[e]
        seen = self.seen[e]
        for k, (sem, v) in deps.items():
            if k == e and (e == "pe" or not SAME_ENGINE_SYNC):
                continue
            if seen.get(k, 0) >= v:
                continue
            if k in self.cnt:
                assert v <= self.cnt[k], "dependency on unsignalled instruction %s %d>%d" % (k, v, self.cnt[k])
            eng.wait_ge(sem, v)
            seen[k] = v

    def _post(self, e, ins, r, w, sig):
        if sig:
            self.cnt[e] += 1
            ins.then_inc(self.sem[e], 1)
            v = self.cnt[e]
        else:
            v = self.cnt[e] + 1
        d = (e, self.sem[e], v)
        for t in r:
            t.r[e] = d
        for t in w:
            t.w = d
            t.r = {}

    def op(self, e, r=(), w=(), sig=True):
        r, w = list(r), list(w)
        ex = [t for t in r if t.excl]
        if ex:
            r = [t for t in r if not t.excl]
            w = w + [t for t in ex if t not in w]
        return _Px(self, e, r, w, sig)

    def pe(self, r=(), w=(), sig=True):
        return self.op("pe", r, w, sig)

    def act(self, r=(), w=()):
        return self.op("act", r, w)

    def dve(self, r=(), w=()):
        return self.op("dve", r, w)

    def pool(self, r=(), w=()):
        return self.op("pool", r, w)

    def dma(self, q, out, in_, r=(), w=()):
        r, w = list(r), list(w)
        self._pre(q, r, w)
        owner = w[0] if w else r[0]
        if owner.ds is None:
            owner.ds = self.dsp[self.dsi % len(self.dsp)]
            self.dsi += 1
        ds = owner.ds
        ins = self.eng[q].dma_start(out=out, in_=in_)
        ds.cnt += 16
        ins.then_inc(ds.sem, 16)
        d = (ds.name, ds.sem, ds.cnt)
        for t in r:
            t.r[ds.name] = d
        for t in w:
            t.w = d
            t.r = {}
        return ins

    def barrier(self):
        evs = [(k, self.sem[k], self.cnt[k]) for k in self.eng if self.cnt[k] > 0]
        evs += [(ds.name, ds.sem, ds.cnt) for ds in self.dsp if ds.cnt > 0]
        evs += self.extra
        for e, eng in self.eng.items():
            seen = self.seen[e]
            for k, sem, v in evs:
                if seen.get(k, 0) >= v:
                    continue
                eng.wait_ge(sem, v)
                seen[k] = v


class Buf:
    def __init__(self, t, name):
        self.h = t
        self.ap = t[:]
        self.t = Tile(name)


class Ring:
    def __init__(self, bufs):
        self.bufs = bufs
        self.i = 0

    def next(self):
        b = self.bufs[self.i % len(self.bufs)]
        self.i += 1
        return b


def build(depth=4, stop_after=None, debug=False):
    nc = bass.Bass("TRN2", target_bir_lowering=False)

    def din(name, shape, dt=F32):
        return nc.dram_tensor(name, shape, dt, kind="ExternalInput")

    def dscr(name, shape, dt=F32, dump=False):
        if debug and dump:
            return nc.dram_tensor(name, shape, dt, kind="ExternalOutput")
        return nc.dram_tensor(name, shape, dt)

    x_in = din("x_in", [T, D])
    cond5 = din("cond5", [P, KC, 5])
    w_ada_sh = din("w_ada_sh", [D, depth * 1536])
    b_ada_sh = din("b_ada_sh", [P, depth * 12])
    selb = din("selb", [P, 4])
    w_in = din("w_in", [depth, D, DIN])
    bgate = din("bgate", [depth, 16])
    mhg = din("mhg", [depth, 1024])
    conv_w = din("conv_w", [depth, P, 3, 8])
    conv_b = din("conv_b", [depth, P, 8])
    w_out = din("w_out", [depth, D, D])
    ln1_g = din("ln1_g", [depth, D])
    ln1_b = din("ln1_b", [depth, D])
    ln2_g = din("ln2_g", [depth, D])
    ln2_b = din("ln2_b", [depth, D])
    w_router = din("w_router", [P, KC, 16])
    b_router = din("b_router", [1, 16])
    w1 = din("w1", [depth, NE, D, FF])
    w3 = din("w3", [depth, NE, D, FF])
    w2 = din("w2", [depth, NE, FF, D])
    ident = din("ident", [P, P])
    masks = din("masks", [2, P, P])
    sel = din("sel", [P, 2])
    out = nc.dram_tensor("out", [2048, D], F32, kind="ExternalOutput")

    xl_d = dscr("xl_d", [T, D], F32, True)
    ccm_in = nc.dram_tensor("ccm_in", [P, depth * 60], F32)
    ccm_out = nc.dram_tensor("ccm_out", [8 * P, depth * 60], F32)
    qT_d = dscr("qT_d", [4, P, T], BF16, True)
    kT_d = dscr("kT_d", [4, P, T], BF16, True)
    k_d = dscr("k_d", [T, 512], BF16, True)
    v_d = dscr("v_d", [T, 1024], BF16, True)
    gates_d = dscr("gates_d", [T, 16], F32, True)
    so_d = dscr("so_d", [T, 1024], BF16, True)
    mixT_d = dscr("mixT_d", [16, P, T], BF16, True)
    h2T_d = dscr("h2T_d", [P, KC, T], BF16, True)
    logit_d = dscr("logit_d", [T, 16], F32, True)
    cc_in = [nc.dram_tensor("cc_in%d" % i, [P, D], F32) for i in range(NTH)]
    cc_out = [nc.dram_tensor("cc_out%d" % i, [2 * P, D], F32) for i in range(NTH)]
    dbg_mod = dscr("dbg_mod", [P, depth * 4 * 16 * 2], F32, True)
    dbg_moe = dscr("dbg_moe", [TH, D], F32, True)
    dbg_gates = dscr("dbg_gates", [P, NTH * 16], F32, True)

    with ExitStack() as st:
        S = Sched(nc, st)
        cc_sem = st.enter_context(nc.semaphore("cc_sem"))
        cc_n = [0]

        uid = [0]

        def sbuf(stk, name, shape, dt):
            uid[0] += 1
            return stk.enter_context(nc.sbuf_tensor("%s_u%d" % (name, uid[0]), shape, dt))

        def mk(stk, name, shape, dt):
            return Buf(sbuf(stk, name, shape, dt), name)

        def ring(stk, name, shape, dt, n):
            return Ring([mk(stk, "%s%d" % (name, i), shape, dt) for i in range(n)])

        banks = [Buf(st.enter_context(nc.psum_tensor("ps%d" % i, [P, 512], F32)), "ps%d" % i) for i in range(8)]
        for b_ in banks:
            b_.t.excl = True
        bank_i = [0]

        def bank():
            b = banks[bank_i[0] % 8]
            bank_i[0] += 1
            return b

        idf = mk(st, "idf", [P, P], F32)
        idb = mk(st, "idb", [P, P], BF16)
        mkf = [mk(st, "mk0", [P, P], F32), mk(st, "mk1", [P, P], F32)]
        ones_col = mk(st, "ones_col", [P, 1], F32)
        ones_row = mk(st, "ones_row", [1, P], F32)
        selt = mk(st, "selt", [P, 2], F32)
        modT = mk(st, "modT", [P, depth, 4, KC, 2], F32)
        stats_r = ring(st, "stats", [P, 32], F32, 3)

        S.dma("sp", out=idf.ap, in_=ident[:, :], w=[idf.t])
        S.dma("sp", out=mkf[0].ap, in_=masks[0, :, :], w=[mkf[0].t])
        S.dma("sp", out=mkf[1].ap, in_=masks[1, :, :], w=[mkf[1].t])
        S.dma("sp", out=selt.ap, in_=sel[:, :], w=[selt.t])
        S.dve([idf.t], [idb.t]).tensor_copy(out=idb.ap, in_=idf.ap)
        S.dve([], [ones_col.t]).memset(ones_col.ap, 1.0)
        S.dve([], [ones_row.t]).memset(ones_row.ap, 1.0)

        def finish():
            S.barrier()

        def ln_stats(xap, tx):
            sb_ = stats_r.next()
            a = sb_.ap
            for k in range(4):
                S.dve([tx], [sb_.t]).bn_stats(a[:, k * 6:(k + 1) * 6], xap[:, k * 512:(k + 1) * 512])
            S.dve([sb_.t], [sb_.t]).bn_aggr(a[:, 24:26], a[:, 0:24])
            S.act([sb_.t], [sb_.t]).activation(out=a[:, 26:27], in_=a[:, 25:26], func=AF.Sqrt, bias=EPS, scale=1.0)
            S.dve([sb_.t], [sb_.t]).reciprocal(a[:, 27:28], a[:, 26:27])
            return a[:, 24:25], a[:, 27:28], sb_.t

        def ln_mod_T(xap, tx, xn_ring, l, kind, r, dst_fn):
            mean, rstd, ts = ln_stats(xap, tx)
            xn = xn_ring.next()
            S.dve([tx, ts], [xn.t]).tensor_scalar(out=xn.ap, in0=xap, scalar1=mean, scalar2=rstd,
                                                  op0=ALU.subtract, op1=ALU.mult)
            for half in range(2):
                b = bank()
                pb = b.ap.bitcast(BF16)
                for cc in range(8):
                    c = half * 8 + cc
                    S.pe([xn.t, idb.t], [b.t], sig=(cc == 7)).transpose(
                        out=pb[:, cc * 128:(cc + 1) * 128], in_=xn.ap[:, c * 128:(c + 1) * 128], identity=idb.ap)
                for cc in range(8):
                    c = half * 8 + cc
                    dap, dt_ = dst_fn(c)
                    S.act([b.t, modT.t], [dt_]).activation(
                        out=dap, in_=pb[:, cc * 128:(cc + 1) * 128], func=AF.Identity,
                        scale=modT.ap[:, l, kind + 1, c, r:r + 1], bias=modT.ap[:, l, kind, c, r:r + 1])

        def ln_affine(xt, g_b, b_b):
            mean, rstd, ts = ln_stats(xt.ap, xt.t)
            S.dve([xt.t, ts], [xt.t]).tensor_scalar(out=xt.ap, in0=xt.ap, scalar1=mean, scalar2=rstd,
                                                    op0=ALU.subtract, op1=ALU.mult)
            S.dve([xt.t, g_b.t], [xt.t]).tensor_tensor(out=xt.ap, in0=xt.ap, in1=g_b.ap, op=ALU.mult)
            S.dve([xt.t, b_b.t], [xt.t]).tensor_tensor(out=xt.ap, in0=xt.ap, in1=b_b.ap, op=ALU.add)

        NCH = depth * 12
        NB = NCH // 4
        modb = mk(st, "modb", [P, depth * 96], F32)
        modc = mk(st, "modc", [P, depth * 96], F32)
        ones_f = mk(st, "ones_f", [P, P], F32)
        dg_r = ring(st, "dg_r", [P, P], F32, 3)
        S.dve([], [ones_f.t]).memset(ones_f.ap, 1.0)
        with ExitStack() as ph:
            cond_f = mk(ph, "cond_f", [P, KC, 5], F32)
            cond_s = mk(ph, "cond_s", [P, KC, 5], F32)
            condT = mk(ph, "condT", [P, KC, 5], BF16)
            bsh = mk(ph, "bsh", [P, NCH], F32)
            selbt = mk(ph, "selbt", [P, 4], F32)
            mod_sh = mk(ph, "mod_sh", [P, NCH, 5], F32)
            modall = mk(ph, "modall", [P, 8, NCH * 5], F32)
            wr0 = ring(ph, "w0r", [P, KC, 512], BF16, 3)
            S.dma("sp", out=cond_f.ap, in_=cond5[:, :, :], w=[cond_f.t])
            S.dma("sp", out=bsh.ap, in_=b_ada_sh[:, :], w=[bsh.t])
            S.dma("sp", out=selbt.ap, in_=selb[:, :], w=[selbt.t])
            S.act([cond_f.t], [cond_s.t]).activation(out=cond_s.ap, in_=cond_f.ap, func=AF.Silu)
            S.dve([cond_s.t], [condT.t]).tensor_copy(out=condT.ap, in_=cond_s.ap)
            for blk in range(NB):
                wb = wr0.next()
                S.dma("pool", out=wb.ap, in_=w_ada_sh[:, blk * 512:(blk + 1) * 512].rearrange("(kc p) n -> p kc n", p=P),
                      w=[wb.t])
                for cc in range(4):
                    cq = blk * 4 + cc
                    b = bank()
                    for kc in range(KC):
                        S.pe([wb.t, condT.t], [b.t], sig=(kc == KC - 1)).matmul(
                            b.ap[:, 0:5], wb.ap[:, kc, cc * 128:(cc + 1) * 128], condT.ap[:, kc, :],
                            start=(kc == 0), stop=(kc == KC - 1))
                    S.dve([b.t, bsh.t], [mod_sh.t]).tensor_scalar(
                        out=mod_sh.ap[:, cq, :], in0=b.ap[:, 0:5], scalar1=bsh.ap[:, cq:cq + 1], scalar2=None, op0=ALU.add)
            S.dma("sp", out=ccm_in[:, :], in_=mod_sh.ap.rearrange("p q r -> p (q r)"), r=[mod_sh.t])
            S._pre("pool", [], [mod_sh.t])
            cc_n[0] += 1
            nc.gpsimd.collective_compute("AllGather", ALU.bypass, replica_groups=[list(range(8))],
                                         ins=[ccm_in.ap().opt()], outs=[ccm_out.ap().opt()]).then_inc(cc_sem, 1)
            S.extra = [("cc", cc_sem, cc_n[0])]
            S.barrier()
            S.dma("sp", out=modall.ap, in_=ccm_out.ap().rearrange("(k p) f -> p k f", p=P), w=[modall.t])
            mv = modall.ap.rearrange("p k (q r) -> p (k q) r", r=5)
            S.dve([modall.t, selbt.t], [modb.t]).tensor_scalar(out=modb.ap, in0=mv[:, :, 0], scalar1=selbt.ap[:, 0:1],
                                                               scalar2=None, op0=ALU.mult)
            for bb_ in range(1, 4):
                S.dve([modall.t, selbt.t, modb.t], [modb.t]).scalar_tensor_tensor(
                    out=modb.ap, in0=mv[:, :, bb_], scalar=selbt.ap[:, bb_:bb_ + 1], in1=modb.ap, op0=ALU.mult, op1=ALU.add)
            S.dve([modall.t], [modc.t]).tensor_copy(out=modc.ap, in_=mv[:, :, 4])
            for l in range(depth):
                for kind, j in ((0, 0), (1, 1), (2, 3), (3, 4)):
                    for r, src in ((0, modb), (1, modc)):
                        q0 = l * 96 + j * 16
                        S.dve([src.t], [modT.t]).tensor_scalar(
                            out=modT.ap[:, l, kind, :, r], in0=src.ap[:, q0:q0 + 16],
                            scalar1=(1.0 if j in (1, 4) else 0.0), scalar2=None, op0=ALU.add)
            if debug:
                S.dma("sp", out=dbg_mod[:, :], in_=modT.ap.rearrange("p l k c r -> p (l k c r)"), r=[modT.t])
            S.barrier()
        if stop_after == "M0":
            finish()
            return nc

        def gate_bcast(dst, l, j, r):
            src = modb if r == 0 else modc
            for qd in range(4):
                b = bank()
                for cc in range(4):
                    q = l * 96 + j * 16 + qd * 4 + cc
                    dg = dg_r.next()
                    S.dve([idf.t, src.t], [dg.t]).tensor_scalar(out=dg.ap, in0=idf.ap, scalar1=src.ap[:, q:q + 1],
                                                                scalar2=None, op0=ALU.mult)
                    S.pe([ones_f.t, dg.t], [b.t]).matmul(b.ap[:, cc * 128:(cc + 1) * 128], ones_f.ap, dg.ap,
                                                         start=True, stop=True)
                S.act([b.t], [dst.t]).copy(out=dst.ap[:, qd * 512:(qd + 1) * 512], in_=b.ap)

        for l in range(depth):
            last = (l == depth - 1)
            xsrc = x_in if l == 0 else xl_d

            with ExitStack() as ph:
                hT = sbuf(ph, "hT", [P, KC, T], BF16)
                t_hT = [Tile("hT%d" % i) for i in range(NT)]
                with ExitStack() as ph1:
                    x_r = ring(ph1, "x_r", [P, D], F32, 3)
                    xn_r = ring(ph1, "xn_r", [P, D], BF16, 3)
                    xns = {}

                    def m1_a(i):
                        xt = x_r.next()
                        S.dma("sp", out=xt.ap, in_=xsrc[i * 128:(i + 1) * 128, :], w=[xt.t])
                        mean, rstd, ts = ln_stats(xt.ap, xt.t)
                        xn = xn_r.next()
                        S.dve([xt.t, ts], [xn.t]).tensor_scalar(out=xn.ap, in0=xt.ap, scalar1=mean, scalar2=rstd,
                                                              op0=ALU.subtract, op1=ALU.mult)
                        xns[i] = xn

                    def m1_b(i):
                        r = 1 if i < 2 else 0
                        xn = xns.pop(i)
                        for half in range(2):
                            b = bank()
                            pb = b.ap.bitcast(BF16)
                            for cc in range(8):
                                c = half * 8 + cc
                                S.pe([xn.t, idb.t], [b.t], sig=(cc == 7)).transpose(
                                    out=pb[:, cc * 128:(cc + 1) * 128], in_=xn.ap[:, c * 128:(c + 1) * 128], identity=idb.ap)
                            for cc in range(8):
                                c = half * 8 + cc
                                S.act([b.t, modT.t], [t_hT[i]]).activation(
                                    out=hT[:, c, i * 128:(i + 1) * 128], in_=pb[:, cc * 128:(cc + 1) * 128],
                                    func=AF.Identity, scale=modT.ap[:, l, 1, c, r:r + 1], bias=modT.ap[:, l, 0, c, r:r + 1])
                    for i in range(NT + 1):
                        if i < NT:
                            m1_a(i)
                        if i >= 1:
                            m1_b(i - 1)
                    S.barrier()
                if stop_after == "M1":
                    S.dma("sp", out=h2T_d[:, :, :], in_=hT[:], r=t_hT)
                    finish()
                    return nc

                wr = ring(ph, "wr", [P, KC, 512], BF16, 3)
                stg_bf = ring(ph, "stg_bf", [P, 512], BF16, 4)
                stg_f = ring(ph, "stg_f", [P, 512], F32, 3)
                gates_sb = mk(ph, "gates_sb", [P, NT, 16], F32)
                bg_b = mk(ph, "bg_b", [P, 16], F32)
                cw = mk(ph, "cw", [P, 3, 8], F32)
                cb = mk(ph, "cb", [P, 8], F32)
                zT = sbuf(ph, "zT", [P, 4, T], BF16)
                bgT = sbuf(ph, "bgT", [P, 4, T], BF16)
                t_z = [Tile("z%d" % i) for i in range(4)]
                t_bg = [Tile("bg%d" % i) for i in range(4)]
                y_r = ring(ph, "y_r", [P, T], F32, 1)
                ym_r = ring(ph, "ym_r", [P, T], BF16, 2)
                S.dma("sp", out=bg_b.ap, in_=bgate[l:l + 1, :].partition_broadcast(P), w=[bg_b.t])
                S.dma("sp", out=cw.ap, in_=conv_w[l, :, :, :], w=[cw.t])
                S.dma("sp", out=cb.ap, in_=conv_b[l, :, :], w=[cb.t])

                TBS = [(0, 512), (512, 512), (1024, 512), (1536, 512), (2048, 256)]

                def load_w(col0, n):
                    wb = wr.next()
                    S.dma("pool", out=wb.ap[:, :, 0:n],
                          in_=w_in[l, :, col0:col0 + n].rearrange("(kc p) n -> p kc n", p=P), w=[wb.t])
                    return wb

                def fm(wb, cc, t0, n):
                    b = bank()
                    rt = [t_hT[i] for i in range(t0 // 128, (t0 + n) // 128)]
                    for kc in range(KC):
                        S.pe([wb.t] + rt, [b.t], sig=(kc == KC - 1)).matmul(
                            b.ap[:, 0:n], wb.ap[:, kc, cc * 128:(cc + 1) * 128], hT[:, kc, t0:t0 + n],
                            start=(kc == 0), stop=(kc == KC - 1))
                    return b

                def tm(wb, i, n):
                    b = bank()
                    for kc in range(KC):
                        S.pe([wb.t, t_hT[i]], [b.t], sig=(kc == KC - 1)).matmul(
                            b.ap[:, 0:n], hT[:, kc, i * 128:(i + 1) * 128], wb.ap[:, kc, 0:n],
                            start=(kc == 0), stop=(kc == KC - 1))
                    return b

                KS = 128.0 ** -0.5
                def conv_half(hf):
                    ub = load_w(3088 + hf * 512, 512)
                    cbk = load_w(5136 + hf * 512, 512)
                    bbk = load_w(4112 + hf * 512, 512)
                    for cc in range(4):
                        for (t0, n) in TBS:
                            pu = fm(ub, cc, t0, n)
                            us = stg_f.next()
                            S.act([pu.t], [us.t]).copy(out=us.ap[:, 0:n], in_=pu.ap[:, 0:n])
                            pc = fm(cbk, cc, t0, n)
                            S.dve([pc.t, us.t], [t_z[cc]]).tensor_tensor(out=zT[:, cc, t0:t0 + n], in0=pc.ap[:, 0:n],
                                                                        in1=us.ap[:, 0:n], op=ALU.mult)
                            pbk = fm(bbk, cc, t0, n)
                            S.act([pbk.t], [t_bg[cc]]).copy(out=bgT[:, cc, t0:t0 + n], in_=pbk.ap[:, 0:n])
                    for cc in range(4):
                        ch = hf * 4 + cc
                        y = y_r.next()
                        z = zT[:, cc, :]
                        S.dve([t_z[cc], cw.t], [y.t]).tensor_scalar(out=y.ap, in0=z, scalar1=cw.ap[:, 1, ch:ch + 1],
                                                                    scalar2=None, op0=ALU.mult)

                        def acc(dst, src, tap):
                            S.dve([t_z[cc], cw.t, y.t], [y.t]).scalar_tensor_tensor(
                                out=dst, in0=src, scalar=cw.ap[:, tap, ch:ch + 1], in1=dst, op0=ALU.mult, op1=ALU.add)
                        acc(y.ap[:, 1:256], z[:, 0:255], 0)
                        acc(y.ap[:, 0:255], z[:, 1:256], 2)
                        if hf == 0:
                            yl = y.ap[:, 256:T].rearrange("p (r c) -> p r c", c=64)
                            zl = zT[:, cc, 256:T].rearrange("p (r c) -> p r c", c=64)
                            acc(yl[:, :, 1:64], zl[:, :, 0:63], 0)
                            acc(yl[:, :, 0:63], zl[:, :, 1:64], 2)
                        else:
                            acc(y.ap[:, 320:T], z[:, 256:T - 64], 0)
                            acc(y.ap[:, 256:T - 64], z[:, 320:T], 2)
                        ym = ym_r.next()
                        S.dve([y.t, t_bg[cc], cb.t], [ym.t]).scalar_tensor_tensor(
                            out=ym.ap, in0=y.ap, scalar=cb.ap[:, ch:ch + 1], in1=bgT[:, cc, :], op0=ALU.add, op1=ALU.mult)
                        S.dma("sp", out=mixT_d[8 + ch, :, :], in_=ym.ap, r=[ym.t])
                conv_half(0)
                wb = load_w(0, 512)
                for cc in range(4):
                    for (t0, n) in TBS:
                        b = fm(wb, cc, t0, n)
                        sg = stg_bf.next()
                        S.act([b.t], [sg.t]).copy(out=sg.ap[:, 0:n], in_=b.ap[:, 0:n])
                        S.dma("sp", out=qT_d[cc, :, t0:t0 + n], in_=sg.ap[:, 0:n], r=[sg.t])
                wb = load_w(512, 512)
                for cc in range(4):
                    for (t0, n) in TBS:
                        b = fm(wb, cc, t0, n)
                        sg = stg_bf.next()
                        S.act([b.t], [sg.t]).mul(out=sg.ap[:, 0:n], in_=b.ap[:, 0:n], mul=KS)
                        S.dma("sp", out=kT_d[cc, :, t0:t0 + n], in_=sg.ap[:, 0:n], r=[sg.t])
                for i in range(NT):
                    b = tm(wb, i, 512)
                    sg = stg_bf.next()
                    S.act([b.t], [sg.t]).mul(out=sg.ap, in_=b.ap, mul=KS)
                    S.dma("sp", out=k_d[i * 128:(i + 1) * 128, :], in_=sg.ap, r=[sg.t])
                for hf in range(2):
                    wb = load_w(1024 + hf * 512, 512)
                    for i in range(NT):
                        b = tm(wb, i, 512)
                        sg = stg_bf.next()
                        S.act([b.t], [sg.t]).copy(out=sg.ap, in_=b.ap)
                        S.dma("sp", out=v_d[i * 128:(i + 1) * 128, hf * 512:(hf + 1) * 512], in_=sg.ap, r=[sg.t])
                wb = load_w(2048, 16)
                for i in range(NT):
                    b = tm(wb, i, 16)
                    S.dve([b.t, bg_b.t], [gates_sb.t]).tensor_tensor(out=gates_sb.ap[:, i, :], in0=b.ap[:, 0:16],
                                                                   in1=bg_b.ap, op=ALU.add)
                S.dma("sp", out=gates_d.ap().rearrange("(i p) c -> p i c", p=P), in_=gates_sb.ap, r=[gates_sb.t])
                for hf in range(2):
                    wb = load_w(2064 + hf * 512, 512)
                    for i in range(NT):
                        b = tm(wb, i, 512)
                        sg = stg_bf.next()
                        S.act([b.t], [sg.t]).activation(out=sg.ap, in_=b.ap, func=AF.Sigmoid)
                        S.dma("sp", out=so_d[i * 128:(i + 1) * 128, hf * 512:(hf + 1) * 512], in_=sg.ap, r=[sg.t])
                conv_half(1)
                S.barrier()
            if stop_after == "M2":
                finish()
                return nc

            with ExitStack() as ph:
                gt = mk(ph, "gt", [P, NT, 16], F32)
                ex = mk(ph, "ex", [P, NT, 8], F32)
                spl = mk(ph, "spl", [P, NT, 8], F32)
                lfd = [mk(ph, "lfd%d" % d, [P, 72], F32) for d in range(2)]
                Bv = [mk(ph, "Bv%d" % d, [P, 72], F32) for d in range(2)]
                bsb = [mk(ph, "bsb%d" % d, [P, 72], F32) for d in range(2)]
                mxc = [mk(ph, "mxc%d" % d, [P, 1], F32) for d in range(2)]
                rows = [mk(ph, "rows%d" % d, [1, 4 * 72 + 4], F32) for d in range(2)]
                gb = [mk(ph, "gb%d" % d, [P, 72], F32) for d in range(2)]
                ev = [mk(ph, "ev%d" % d, [P, 72], F32) for d in range(2)]
                thr = [mk(ph, "thr%d" % d, [P, 72], F32) for d in range(2)]
                tmp72 = [mk(ph, "tmp72_%d" % d, [P, 72], F32) for d in range(2)]
                g_b = mk(ph, "g_b", [P, 1024], F32)
                S.dma("sp", out=gt.ap, in_=gates_d.ap().rearrange("(i p) c -> p i c", p=P), w=[gt.t])
                S.dma("sp", out=g_b.ap, in_=mhg[l:l + 1, :].partition_broadcast(P), w=[g_b.t])
                S.act([gt.t], [ex.t]).activation(out=ex.ap, in_=gt.ap[:, :, 8:16], func=AF.Exp, scale=-1.0)
                S.act([ex.t], [spl.t]).activation(out=spl.ap, in_=ex.ap, func=AF.Ln, bias=1.0)
                for d in range(2):
                    S.dve([spl.t], [lfd[d].t]).tensor_scalar(
                        out=lfd[d].ap.rearrange("p (c h) -> p c h", h=4), in0=spl.ap[:, :, d * 4:(d + 1) * 4],
                        scalar1=-1.0, scalar2=None, op0=ALU.mult)
                    pb_ = bank()
                    S.pe([mkf[d].t, lfd[d].t], [pb_.t]).matmul(pb_.ap[:, 0:72], mkf[d].ap, lfd[d].ap, start=True, stop=True)
                    S.dve([gt.t, pb_.t], [Bv[d].t]).tensor_tensor(
                        out=Bv[d].ap.rearrange("p (c h) -> p c h", h=4), in0=gt.ap[:, :, d * 4:(d + 1) * 4],
                        in1=pb_.ap[:, 0:72].rearrange("p (c h) -> p c h", h=4), op=ALU.subtract)
                    S.act([pb_.t], [bsb[d].t]).copy(out=bsb[d].ap, in_=pb_.ap[:, 0:72])
                    pe_ = bank()
                    S.pe([ones_col.t, lfd[d].t], [pe_.t]).matmul(pe_.ap[0:1, 0:72], ones_col.ap, lfd[d].ap,
                                                                 start=True, stop=True)
                    rw = rows[d]
                    S.act([pe_.t], [rw.t]).copy(out=rw.ap[0:1, 0:72], in_=pe_.ap[0:1, 0:72])
                    pt_ = bank()
                    S.pe([Bv[d].t, idf.t], [pt_.t]).transpose(out=pt_.ap[0:72, 0:128], in_=Bv[d].ap, identity=idf.ap)
                    S.dve([pt_.t], [mxc[d].t]).tensor_reduce(out=mxc[d].ap[0:72, 0:1], in_=pt_.ap[0:72, 0:128],
                                                             axis=AX.X, op=ALU.max)
                    pm_ = bank()
                    S.pe([mxc[d].t, idf.t], [pm_.t]).transpose(out=pm_.ap[0:1, 0:72], in_=mxc[d].ap[0:72, 0:1],
                                                               identity=idf.ap[0:72, 0:72])
                    S.act([pm_.t], [rw.t]).copy(out=rw.ap[0:1, 72:144], in_=pm_.ap[0:1, 0:72])
                    mst = rw.ap[0:1, 288:292]
                    S.dve([], [rw.t]).memset(mst, 0.0)
                    for c in ORDER[d]:
                        bend_c = rw.ap[0:1, c * 4:c * 4 + 4]
                        mx_c = rw.ap[0:1, 72 + c * 4:72 + c * 4 + 4]
                        M_c = rw.ap[0:1, 144 + c * 4:144 + c * 4 + 4]
                        g_c = rw.ap[0:1, 216 + c * 4:216 + c * 4 + 4]
                        S.dve([rw.t], [rw.t]).tensor_tensor(out=M_c, in0=mst, in1=mx_c, op=ALU.max)
                        S.dve([rw.t], [rw.t]).tensor_tensor(out=g_c, in0=mst, in1=M_c, op=ALU.subtract)
                        S.dve([rw.t], [rw.t]).tensor_tensor(out=mst, in0=bend_c, in1=M_c, op=ALU.add)
                    S.act([rw.t], [rw.t]).activation(out=rw.ap[0:1, 216:288], in_=rw.ap[0:1, 216:288], func=AF.Exp)
                    pM = bank()
                    S.pe([ones_row.t, rw.t], [pM.t]).matmul(pM.ap[:, 0:72], ones_row.ap, rw.ap[0:1, 144:216],
                                                            start=True, stop=True)
                    pG = bank()
                    S.pe([ones_row.t, rw.t], [pG.t]).matmul(pG.ap[:, 0:72], ones_row.ap, rw.ap[0:1, 216:288],
                                                            start=True, stop=True)
                    S.act([pG.t], [gb[d].t]).copy(out=gb[d].ap, in_=pG.ap[:, 0:72])
                    S.dve([Bv[d].t, pM.t], [tmp72[d].t]).tensor_tensor(out=tmp72[d].ap, in0=Bv[d].ap, in1=pM.ap[:, 0:72],
                                                                       op=ALU.subtract)
                    S.act([tmp72[d].t], [ev[d].t]).activation(out=ev[d].ap, in_=tmp72[d].ap, func=AF.Exp)
                    S.dve([bsb[d].t, pM.t], [tmp72[d].t]).tensor_tensor(out=tmp72[d].ap, in0=bsb[d].ap, in1=pM.ap[:, 0:72],
                                                                        op=ALU.add)
                    S.act([tmp72[d].t], [thr[d].t]).activation(out=thr[d].ap, in_=tmp72[d].ap, func=AF.Exp, scale=-1.0)

                NHB = 2
                qTh = [mk(ph, "qTh%d" % i, [P, T], BF16) for i in range(NHB)]
                kTh = [mk(ph, "kTh%d" % i, [P, T], BF16) for i in range(NHB)]
                kh = [mk(ph, "kh%d" % i, [P, NT, 128], BF16) for i in range(NHB)]
                v1h = [mk(ph, "v1h%d" % i, [P, NT, 257], BF16) for i in range(NHB)]
                soh = [mk(ph, "soh%d" % i, [P, NT, 256], BF16) for i in range(NHB)]
                mixh = [mk(ph, "mixh%d" % i, [P, 2, T], BF16) for i in range(NHB)]
                A_ = [[mk(ph, "A%d_%d" % (i, d), [P, 257], F32) for d in range(2)] for i in range(NHB)]
                Ab = [[mk(ph, "Ab%d_%d" % (i, d), [P, 257], BF16) for d in range(2)] for i in range(NHB)]
                raw = [[sbuf(ph, "raw%d_%d" % (i, d), [P, NT, 257], F32) for d in range(2)] for i in range(NHB)]
                t_raw = [[[Tile("raw%d_%d_%d" % (i, d, c)) for c in range(NT)] for d in range(2)] for i in range(NHB)]
                rin = [[mk(ph, "rin%d_%d" % (i, d), [P, 3 * NT], F32) for d in range(2)] for i in range(NHB)]
                sw_r = ring(ph, "sw_r", [P, P], BF16, 12)
                ke_r = ring(ph, "ke_r", [P, P], BF16, 12)
                hs_r = ring(ph, "hs_r", [P, 256], F32, 3)
                gs_r = ring(ph, "gs_r", [P, 256], F32, 4)
                mt_r = ring(ph, "mt_r", [P, 256], BF16, 4)
                junk = mk(ph, "junk", [P, 256], F32)
                ss = mk(ph, "ss", [P, 3 * NT], F32)
                for grp in range(2):
                    for hh in range(NHB):
                        h = grp * NHB + hh
                        S.dma("sp", out=qTh[hh].ap, in_=qT_d[h, :, :], w=[qTh[hh].t])
                        S.dma("sp", out=kTh[hh].ap, in_=kT_d[h, :, :], w=[kTh[hh].t])
                        S.dma("sp", out=kh[hh].ap, in_=k_d[:, h * 128:(h + 1) * 128].rearrange("(c p) k -> p c k", p=P),
                              w=[kh[hh].t])
                        S.dve([], [v1h[hh].t]).memset(v1h[hh].ap[:, :, 256:257], 1.0)
                        S.dma("sp", out=v1h[hh].ap[:, :, 0:256],
                              in_=v_d[:, h * 256:(h + 1) * 256].rearrange("(c p) v -> p c v", p=P), w=[v1h[hh].t])
                        S.dma("sp", out=soh[hh].ap, in_=so_d[:, h * 256:(h + 1) * 256].rearrange("(c p) v -> p c v", p=P),
                              w=[soh[hh].t])
                    sws = {}

                    def scan_a(s_):
                        for hh in range(NHB):
                            h = grp * NHB + hh
                            for d in range(2):
                                c = ORDER[d][s_]
                                col = c * 4 + h
                                tsl = slice(c * 128, (c + 1) * 128)
                                pS = bank()
                                S.pe([kTh[hh].t, qTh[hh].t], [pS.t]).matmul(pS.ap[:, 0:128], kTh[hh].ap[:, tsl],
                                                                          qTh[hh].ap[:, tsl], start=True, stop=True)
                                sw = sw_r.next()
                                S.dve([pS.t, ev[d].t, mkf[d].t], [sw.t]).scalar_tensor_tensor(
                                    out=sw.ap, in0=pS.ap[:, 0:128], scalar=ev[d].ap[:, col:col + 1], in1=mkf[d].ap,
                                    op0=ALU.mult, op1=ALU.mult)
                                sws[(s_, hh, d)] = sw
                                nxt = (ORDER[d][s_ + 1] * 4 + h) if s_ < NT - 1 else None
                                if nxt is not None:
                                    ke = ke_r.next()
                                    S.pool([kh[hh].t, ev[d].t, gb[d].t], [ke.t]).tensor_scalar(
                                        out=ke.ap, in0=kh[hh].ap[:, c, :], scalar1=ev[d].ap[:, col:col + 1],
                                        scalar2=gb[d].ap[:, nxt:nxt + 1], op0=ALU.mult, op1=ALU.mult)
                                    sws[(s_, hh, d, "ke")] = ke

                    def scan_b(s_):
                        for hh in range(NHB):
                            h = grp * NHB + hh
                            for d in range(2):
                                c = ORDER[d][s_]
                                nxt = (ORDER[d][s_ + 1] * 4 + h) if s_ < NT - 1 else None
                                tsl = slice(c * 128, (c + 1) * 128)
                                A, Abf = A_[hh][d], Ab[hh][d]
                                sw = sws.pop((s_, hh, d))
                                pN = bank()
                                if s_ == 0:
                                    S.pe([sw.t, v1h[hh].t], [pN.t]).matmul(pN.ap[:, 0:257], sw.ap, v1h[hh].ap[:, c, :],
                                                                          start=True, stop=True)
                                else:
                                    S.pe([sw.t, v1h[hh].t], [pN.t], sig=False).matmul(
                                        pN.ap[:, 0:257], sw.ap, v1h[hh].ap[:, c, :], start=True, stop=False)
                                    S.pe([qTh[hh].t, Abf.t], [pN.t]).matmul(pN.ap[:, 0:257], qTh[hh].ap[:, tsl], Abf.ap,
                                                                           start=False, stop=True)
                                S.act([pN.t], [t_raw[hh][d][c]]).copy(out=raw[hh][d][:, c, :], in_=pN.ap[:, 0:257])
                                if nxt is not None:
                                    ke = sws.pop((s_, hh, d, "ke"))
                                    pC = bank()
                                    S.pe([ke.t, v1h[hh].t], [pC.t]).matmul(pC.ap[:, 0:257], ke.ap, v1h[hh].ap[:, c, :],
                                                                          start=True, stop=True)
                                    if s_ == 0:
                                        S.dve([pC.t], [A.t]).tensor_copy(out=A.ap, in_=pC.ap[:, 0:257])
                                    else:
                                        S.dve([A.t, gb[d].t, pC.t], [A.t]).scalar_tensor_tensor(
                                            out=A.ap, in0=A.ap, scalar=gb[d].ap[:, nxt:nxt + 1], in1=pC.ap[:, 0:257],
                                            op0=ALU.mult, op1=ALU.add)
                                    S.act([A.t], [Abf.t]).copy(out=Abf.ap, in_=A.ap)
                    for s_ in range(NT + 1):
                        if s_ < NT:
                            scan_a(s_)
                        if s_ >= 1:
                            scan_b(s_ - 1)
                    for hh in range(NHB):
                        h = grp * NHB + hh
                        for d in range(2):
                            rn = rin[hh][d]
                            den = raw[hh][d][:, :, 256]
                            thr_h = thr[d].ap.rearrange("p (c h) -> p c h", h=4)[:, :, h]
                            rd = t_raw[hh][d]
                            S.dve(rd, [rn.t]).tensor_scalar(out=rn.ap[:, 0:NT], in0=den, scalar1=-1.0, scalar2=None,
                                                            op0=ALU.mult)
                            S.dve(rd + [thr[d].t], [rn.t]).tensor_tensor(out=rn.ap[:, NT:2 * NT], in0=den, in1=thr_h,
                                                                         op=ALU.max)
                            S.dve([rn.t], [rn.t]).tensor_tensor(out=rn.ap[:, NT:2 * NT], in0=rn.ap[:, NT:2 * NT],
                                                                in1=rn.ap[:, 0:NT], op=ALU.max)
                            S.dve([rn.t], [rn.t]).tensor_scalar(out=rn.ap[:, NT:2 * NT], in0=rn.ap[:, NT:2 * NT],
                                                                scalar1=1e-30, scalar2=None, op0=ALU.max)
                            S.dve([rn.t], [rn.t]).reciprocal(rn.ap[:, 2 * NT:3 * NT], rn.ap[:, NT:2 * NT])
                        mh = mixh[hh]
                        r0 = raw[hh][0]
                        for c in range(NT):
                            S.dve([rin[hh][0].t], [t_raw[hh][0][c]]).tensor_scalar(
                                out=r0[:, c, 0:256], in0=r0[:, c, 0:256], scalar1=rin[hh][0].ap[:, 2 * NT + c:2 * NT + c + 1],
                                scalar2=None, op0=ALU.mult)
                            S.dve([t_raw[hh][1][c], rin[hh][1].t], [t_raw[hh][0][c]]).scalar_tensor_tensor(
                                out=r0[:, c, 0:256], in0=raw[hh][1][:, c, 0:256],
                                scalar=rin[hh][1].ap[:, 2 * NT + c:2 * NT + c + 1], in1=r0[:, c, 0:256],
                                op0=ALU.mult, op1=ALU.add)
                        for c in range(NT):
                            S.act([t_raw[hh][0][c]], [junk.t, ss.t]).activation(out=junk.ap, in_=r0[:, c, 0:256], func=AF.Square,
                                                                              accum_out=ss.ap[:, c:c + 1])
                        S.act([ss.t], [ss.t]).activation(out=ss.ap[:, NT:2 * NT], in_=ss.ap[:, 0:NT],
                                                         func=AF.Sqrt, scale=1.0 / 256.0, bias=EPS)
                        S.dve([ss.t], [ss.t]).reciprocal(ss.ap[:, 2 * NT:3 * NT], ss.ap[:, NT:2 * NT])
                        for c in range(NT):
                            gs = gs_r.next()
                            S.pool([soh[hh].t, g_b.t], [gs.t]).tensor_tensor(out=gs.ap, in0=soh[hh].ap[:, c, :],
                                                                            in1=g_b.ap[:, h * 256:(h + 1) * 256], op=ALU.mult)
                            mt = mt_r.next()
                            S.dve([t_raw[hh][0][c], ss.t, gs.t], [mt.t]).scalar_tensor_tensor(
                                out=mt.ap, in0=r0[:, c, 0:256], scalar=ss.ap[:, 2 * NT + c:2 * NT + c + 1], in1=gs.ap,
                                op0=ALU.mult, op1=ALU.mult)
                            pT = bank()
                            pbT = pT.ap.bitcast(BF16)
                            for vv in range(2):
                                S.pe([mt.t, idb.t], [pT.t], sig=(vv == 1)).transpose(
                                    out=pbT[:, vv * 128:(vv + 1) * 128], in_=mt.ap[:, vv * 128:(vv + 1) * 128],
                                    identity=idb.ap)
                            for vv in range(2):
                                S.act([pT.t], [mh.t]).copy(out=mh.ap[:, vv, c * 128:(c + 1) * 128],
                                                           in_=pbT[:, vv * 128:(vv + 1) * 128])
                        for vv in range(2):
                            S.dma("sp", out=mixT_d[h * 2 + vv, :, :], in_=mh.ap[:, vv, :], r=[mh.t])
                S.barrier()
            if stop_after == "M3":
                finish()
                return nc

            with ExitStack() as ph:
                wo = sbuf(ph, "wo", [P, KC, D], BF16)
                t_wo = [Tile("wo%d" % i) for i in range(4)]
                g1b = [mk(ph, "g1b%d" % r, [P, D], F32) for r in range(2)]
                lg = mk(ph, "lg", [P, D], F32)
                lb = mk(ph, "lb", [P, D], F32)
                x_r = ring(ph, "x6_r", [P, D], F32, 3)
                tmp_r = ring(ph, "tmp_r", [P, D], F32, 2)
                xn_r = ring(ph, "xn6_r", [P, D], BF16, 2)
                mix_r = ring(ph, "mix_r", [P, KC, 128], BF16, 2)
                h2_r = ring(ph, "h2_r", [P, KC, 128], BF16, 2)
                xnf_r = ring(ph, "xnf_r", [P, D], F32, 2)
                h2f_r = ring(ph, "h2f_r", [P, KC, 128], F32, 2)
                wrf = mk(ph, "wrf", [P, KC, 16], F32)
                lg_sb = mk(ph, "lg_sb", [P, NT, 16], F32)
                S.dma("sp", out=wrf.ap, in_=w_router[:, :, :], w=[wrf.t])
                for cbk in range(4):
                    S.dma("pool", out=wo[:, :, cbk * 512:(cbk + 1) * 512],
                          in_=w_out[l, :, cbk * 512:(cbk + 1) * 512].rearrange("(kc p) n -> p kc n", p=P), w=[t_wo[cbk]])
                for r in range(2):
                    gate_bcast(g1b[r], l, 2, r)
                S.dma("sp", out=lg.ap, in_=ln1_g[l:l + 1, :].partition_broadcast(P), w=[lg.t])
                S.dma("sp", out=lb.ap, in_=ln1_b[l:l + 1, :].partition_broadcast(P), w=[lb.t])
                for i in range(NT):
                    r = 1 if i < 2 else 0
                    rs_ = slice(i * 128, (i + 1) * 128)
                    mx = mix_r.next()
                    S.dma("sp", out=mx.ap, in_=mixT_d[:, :, rs_].rearrange("c p t -> p c t"), w=[mx.t])
                    xt = x_r.next()
                    S.dma("sp", out=xt.ap, in_=xsrc[rs_, :], w=[xt.t])
                    bks = [bank() for _ in range(4)]
                    for cbk in range(4):
                        for kc in range(KC):
                            S.pe([mx.t, t_wo[cbk]], [bks[cbk].t], sig=(kc == KC - 1)).matmul(
                                bks[cbk].ap, mx.ap[:, kc, :], wo[:, kc, cbk * 512:(cbk + 1) * 512],
                                start=(kc == 0), stop=(kc == KC - 1))
                    tmp = tmp_r.next()
                    for cbk in range(4):
                        cs_ = slice(cbk * 512, (cbk + 1) * 512)
                        S.dve([bks[cbk].t, g1b[r].t], [tmp.t]).tensor_tensor(out=tmp.ap[:, cs_], in0=bks[cbk].ap,
                                                                            in1=g1b[r].ap[:, cs_], op=ALU.mult)
                    S.dve([xt.t, tmp.t], [xt.t]).scalar_tensor_tensor(out=xt.ap, in0=xt.ap, scalar=ALPHA, in1=tmp.ap,
                                                                      op0=ALU.mult, op1=ALU.add)
                    ln_affine(xt, lg, lb)
                    S.dma("sp", out=xl_d[rs_, :], in_=xt.ap, r=[xt.t])
                    h2 = h2_r.next()
                    mean2, rstd2, ts2 = ln_stats(xt.ap, xt.t)
                    xnf = xnf_r.next()
                    S.dve([xt.t, ts2], [xnf.t]).tensor_scalar(out=xnf.ap, in0=xt.ap, scalar1=mean2, scalar2=rstd2,
                                                              op0=ALU.subtract, op1=ALU.mult)
                    xn = xn_r.next()
                    S.pool([xnf.t], [xn.t]).tensor_copy(out=xn.ap, in_=xnf.ap)
                    for half in range(2):
                        b = bank()
                        pb = b.ap.bitcast(BF16)
                        for cc in range(8):
                            c = half * 8 + cc
                            S.pe([xn.t, idb.t], [b.t], sig=(cc == 7)).transpose(
                                out=pb[:, cc * 128:(cc + 1) * 128], in_=xn.ap[:, c * 128:(c + 1) * 128], identity=idb.ap)
                        for cc in range(8):
                            c = half * 8 + cc
                            S.act([b.t, modT.t], [h2.t]).activation(
                                out=h2.ap[:, c, :], in_=pb[:, cc * 128:(cc + 1) * 128], func=AF.Identity,
                                scale=modT.ap[:, l, 3, c, r:r + 1], bias=modT.ap[:, l, 2, c, r:r + 1])
                    S.dma("sp", out=h2T_d[:, :, rs_], in_=h2.ap, r=[h2.t])
                    h2f = h2f_r.next()
                    for qd in range(4):
                        b = bank()
                        for cc in range(4):
                            c = qd * 4 + cc
                            S.pe([xnf.t, idf.t], [b.t], sig=(cc == 3)).transpose(
                                out=b.ap[:, cc * 128:(cc + 1) * 128], in_=xnf.ap[:, c * 128:(c + 1) * 128], identity=idf.ap)
                        for cc in range(4):
                            c = qd * 4 + cc
                            S.act([b.t, modT.t], [h2f.t]).activation(
                                out=h2f.ap[:, c, :], in_=b.ap[:, cc * 128:(cc + 1) * 128], func=AF.Identity,
                                scale=modT.ap[:, l, 3, c, r:r + 1], bias=modT.ap[:, l, 2, c, r:r + 1])
                    pl = bank()
                    for kc in range(KC):
                        S.pe([h2f.t, wrf.t], [pl.t], sig=(kc == KC - 1)).matmul(
                            pl.ap[:, 0:16], h2f.ap[:, kc, :], wrf.ap[:, kc, :], start=(kc == 0), stop=(kc == KC - 1))
                    S.act([pl.t], [lg_sb.t]).copy(out=lg_sb.ap[:, i, :], in_=pl.ap[:, 0:16])
                S.dma("sp", out=logit_d.ap().rearrange("(i p) e -> p i e", p=P), in_=lg_sb.ap, r=[lg_sb.t])
                S.barrier()
            if stop_after == "M6":
                finish()
                return nc

            with ExitStack() as ph:
                h2s = sbuf(ph, "h2s", [P, KC, TH], BF16)
                t_h2s = [Tile("h2s%d" % i) for i in range(KC)]
                acc = sbuf(ph, "acc", [P, NTH, D], F32)
                t_acc = [Tile("acc%d" % i) for i in range(NTH)]
                gates = mk(ph, "gates", [P, NTH, 16], F32)
                lgt = mk(ph, "lgt", [P, 2, NTH, 16], F32)
                lgs = mk(ph, "lgs", [P, NTH, 16], F32)
                brb = mk(ph, "brb", [P, 16], F32)
                rt_r = ring(ph, "rt_r", [P, 96], F32, 2)
                with ExitStack() as ph2:
                    ld_r = ring(ph2, "ld_r", [P, 2, TH], BF16, 2)
                    tb_r = ring(ph2, "tb_r", [P, TH], F32, 2)
                    for kc in range(KC):
                        a = ld_r.next()
                        S.dma("sp", out=a.ap, in_=h2T_d[:, kc, :].rearrange("p (j t) -> p j t", j=2), w=[a.t])
                        tb_ = tb_r.next()
                        S.dve([a.t, selt.t], [tb_.t]).tensor_scalar(out=tb_.ap, in0=a.ap[:, 0, :], scalar1=selt.ap[:, 0:1],
                                                                    scalar2=None, op0=ALU.mult)
                        S.dve([a.t, selt.t, tb_.t], [t_h2s[kc]]).scalar_tensor_tensor(
                            out=h2s[:, kc, :], in0=a.ap[:, 1, :], scalar=selt.ap[:, 1:2], in1=tb_.ap,
                            op0=ALU.mult, op1=ALU.add)
                    S.dma("sp", out=lgt.ap, in_=logit_d.ap().rearrange("(j i p) e -> p j i e", j=2, p=P), w=[lgt.t])
                    S.dve([lgt.t, selt.t], [lgs.t]).tensor_scalar(out=lgs.ap, in0=lgt.ap[:, 0, :, :], scalar1=selt.ap[:, 0:1],
                                                                  scalar2=None, op0=ALU.mult)
                    S.dve([lgt.t, selt.t, lgs.t], [lgs.t]).scalar_tensor_tensor(
                        out=lgs.ap, in0=lgt.ap[:, 1, :, :], scalar=selt.ap[:, 1:2], in1=lgs.ap, op0=ALU.mult, op1=ALU.add)
                    S.dma("sp", out=brb.ap, in_=b_router[0:1, :].partition_broadcast(P), w=[brb.t])
                    for i in range(NTH):
                        S.pool([], [t_acc[i]]).memset(acc[:, i, :], 0.0)
                    for i in range(NTH):
                        rt = rt_r.next()
                        a = rt.ap
                        s_ = a[:, 0:16]
                        sb_ = a[:, 16:32]
                        sb2 = a[:, 32:48]
                        m1 = a[:, 48:52]
                        m2 = a[:, 52:56]
                        gsc = a[:, 56:60]
                        gmx = a[:, 60:61]
                        geq = a[:, 61:65]
                        selm = a[:, 65:81]
                        den = a[:, 81:82]
                        rden = a[:, 82:83]
                        S.act([lgs.t], [rt.t]).activation(out=s_, in_=lgs.ap[:, i, :], func=AF.Sigmoid)
                        S.dve([rt.t, brb.t], [rt.t]).tensor_tensor(out=sb_, in0=s_, in1=brb.ap, op=ALU.add)
                        S.dve([rt.t], [rt.t]).tensor_reduce(out=m1, in_=sb_.rearrange("p (g e) -> p g e", e=4),
                                                            axis=AX.X, op=ALU.max)
                        for g in range(4):
                            gsl = slice(g * 4, g * 4 + 4)
                            S.dve([rt.t], [rt.t]).tensor_scalar(out=sb2[:, gsl], in0=sb_[:, gsl], scalar1=m1[:, g:g + 1],
                                                                scalar2=-1e9, op0=ALU.is_equal, op1=ALU.mult)
                        S.dve([rt.t], [rt.t]).tensor_tensor(out=sb2, in0=sb2, in1=sb_, op=ALU.add)
                        S.dve([rt.t], [rt.t]).tensor_reduce(out=m2, in_=sb2.rearrange("p (g e) -> p g e", e=4),
                                                            axis=AX.X, op=ALU.max)
                        S.dve([rt.t], [rt.t]).tensor_tensor(out=gsc, in0=m1, in1=m2, op=ALU.add)
                        S.dve([rt.t], [rt.t]).tensor_reduce(out=gmx, in_=gsc, axis=AX.X, op=ALU.max)
                        S.dve([rt.t], [rt.t]).tensor_scalar(out=geq, in0=gsc, scalar1=gmx, scalar2=None, op0=ALU.is_equal)
                        for g in range(4):
                            gsl = slice(g * 4, g * 4 + 4)
                            S.dve([rt.t], [rt.t]).tensor_scalar(out=selm[:, gsl], in0=sb_[:, gsl], scalar1=m2[:, g:g + 1],
                                                                scalar2=geq[:, g:g + 1], op0=ALU.is_ge, op1=ALU.mult)
                        S.dve([rt.t], [rt.t]).tensor_tensor(out=selm, in0=selm, in1=s_, op=ALU.mult)
                        S.dve([rt.t], [rt.t]).tensor_reduce(out=den, in_=selm, axis=AX.X, op=ALU.add)
                        S.dve([rt.t], [rt.t]).reciprocal(rden, den)
                        S.dve([rt.t], [gates.t]).tensor_scalar(out=gates.ap[:, i, :], in0=selm, scalar1=rden, scalar2=None,
                                                               op0=ALU.mult)
                    S.barrier()
                if debug:
                    S.dma("sp", out=dbg_gates[:, :], in_=gates.ap.rearrange("p i e -> p (i e)"), r=[gates.t])
                aT = sbuf(ph, "aT", [P, 8, TH], BF16)
                t_aT = [Tile("aT%d" % i) for i in range(8)]
                er = ring(ph, "er", [P, KC, 512], BF16, 4)
                sl_r = ring(ph, "sl_r", [P, 512], BF16, 3)
                TBM = [(0, 512), (512, 512), (1024, 128)]
                for e in range(NE):
                    for fh in range(2):
                        s1 = er.next()
                        S.dma("pool", out=s1.ap, in_=w1[l, e, :, fh * 512:(fh + 1) * 512].rearrange("(kc p) n -> p kc n", p=P),
                              w=[s1.t])
                        s3 = er.next()
                        S.dma("pool", out=s3.ap, in_=w3[l, e, :, fh * 512:(fh + 1) * 512].rearrange("(kc p) n -> p kc n", p=P),
                              w=[s3.t])
                        for fcl in range(4):
                            fc = fh * 4 + fcl
                            for (t0, n) in TBM:
                                p1 = bank()
                                for kc in range(KC):
                                    S.pe([s1.t, t_h2s[kc]], [p1.t], sig=(kc == KC - 1)).matmul(
                                        p1.ap[:, 0:n], s1.ap[:, kc, fcl * 128:(fcl + 1) * 128], h2s[:, kc, t0:t0 + n],
                                        start=(kc == 0), stop=(kc == KC - 1))
                                p3 = bank()
                                for kc in range(KC):
                                    S.pe([s3.t, t_h2s[kc]], [p3.t], sig=(kc == KC - 1)).matmul(
                                        p3.ap[:, 0:n], s3.ap[:, kc, fcl * 128:(fcl + 1) * 128], h2s[:, kc, t0:t0 + n],
                                        start=(kc == 0), stop=(kc == KC - 1))
                                sl = sl_r.next()
                                S.act([p1.t], [sl.t]).activation(out=sl.ap[:, 0:n], in_=p1.ap[:, 0:n], func=AF.Silu)
                                S.dve([p3.t, sl.t], [t_aT[fc]]).tensor_tensor(out=aT[:, fc, t0:t0 + n], in0=p3.ap[:, 0:n],
                                                                             in1=sl.ap[:, 0:n], op=ALU.mult)
                    for dh in range(2):
                        s2 = er.next()
                        s2v = s2.ap.rearrange("p a b -> p (a b)").rearrange("p (f n) -> p f n", n=1024)
                        S.dma("pool", out=s2v, in_=w2[l, e, :, dh * 1024:(dh + 1) * 1024].rearrange("(f p) n -> p f n", p=P),
                              w=[s2.t])
                        for i in range(NTH):
                            for cbk in range(2):
                                py = bank()
                                for fc in range(8):
                                    S.pe([t_aT[fc], s2.t], [py.t], sig=(fc == 7)).matmul(
                                        py.ap, aT[:, fc, i * 128:(i + 1) * 128], s2v[:, fc, cbk * 512:(cbk + 1) * 512],
                                        start=(fc == 0), stop=(fc == 7))
                                c0 = dh * 1024 + cbk * 512
                                S.dve([py.t, gates.t, t_acc[i]], [t_acc[i]]).scalar_tensor_tensor(
                                    out=acc[:, i, c0:c0 + 512], in0=py.ap, scalar=gates.ap[:, i, e:e + 1],
                                    in1=acc[:, i, c0:c0 + 512], op0=ALU.mult, op1=ALU.add)
                for i in range(NTH):
                    S.dma("sp", out=cc_in[i][:, :], in_=acc[:, i, :], r=[t_acc[i]])
                    if debug:
                        S.dma("sp", out=dbg_moe[i * 128:(i + 1) * 128, :], in_=acc[:, i, :], r=[t_acc[i]])
                if stop_after == "M8pre":
                    finish()
                    return nc
                S._pre("pool", [], t_acc)
                for i in range(NTH):
                    cc_n[0] += 1
                    nc.gpsimd.collective_compute("AllGather", ALU.bypass, replica_groups=RG,
                                                 ins=[cc_in[i].ap().opt()], outs=[cc_out[i].ap().opt()]).then_inc(cc_sem, 1)
                S.extra = [("cc", cc_sem, cc_n[0])]
                S.barrier()
            if stop_after == "M8":
                finish()
                return nc

            with ExitStack() as ph:
                g2b = [mk(ph, "g2b%d" % r, [P, D], F32) for r in range(2)]
                lg = mk(ph, "lg2", [P, D], F32)
                lb = mk(ph, "lb2", [P, D], F32)
                x_r = ring(ph, "x9_r", [P, D], F32, 3)
                mo_r = ring(ph, "mo_r", [P, D], F32, 3)
                for r in range(2):
                    gate_bcast(g2b[r], l, 5, r)
                S.dma("sp", out=lg.ap, in_=ln2_g[l:l + 1, :].partition_broadcast(P), w=[lg.t])
                S.dma("sp", out=lb.ap, in_=ln2_b[l:l + 1, :].partition_broadcast(P), w=[lb.t])
                for i in range(NT):
                    r = 1 if i < 2 else 0
                    rs_ = slice(i * 128, (i + 1) * 128)
                    xt = x_r.next()
                    S.dma("sp", out=xt.ap, in_=xl_d[rs_, :], w=[xt.t])
                    mo = mo_r.next()
                    S.dma("sp", out=mo.ap, in_=(cc_out[i][0:P, :] if i < NTH else cc_out[i - NTH][P:2 * P, :]), w=[mo.t])
                    S.dve([mo.t, g2b[r].t], [mo.t]).tensor_tensor(out=mo.ap, in0=mo.ap, in1=g2b[r].ap, op=ALU.mult)
                    S.dve([xt.t, mo.t], [xt.t]).scalar_tensor_tensor(out=xt.ap, in0=xt.ap, scalar=ALPHA, in1=mo.ap,
                                                                     op0=ALU.mult, op1=ALU.add)
                    ln_affine(xt, lg, lb)
                    S.dma("sp", out=xl_d[rs_, :], in_=xt.ap, r=[xt.t])
                    if last and i >= 2:
                        S.dma("sp", out=out[(i - 2) * 128:(i - 1) * 128, :], in_=xt.ap, r=[xt.t])
                S.barrier()
        finish()
    return nc


def make_inputs(inp, depth=4, cores=range(8)):
    f = np.float32
    x = np.asarray(inp["x"], f)
    c = np.asarray(inp["c"], f)
    ctx = np.asarray(inp["ctx"], f)
    c_ctx = np.asarray(inp["c_ctx"], f)
    b_ada = np.ascontiguousarray(np.asarray(inp["b_ada"], f)[:depth])
    conv_w = np.asarray(inp["conv_w"], f)[:depth]
    conv_b = np.asarray(inp["conv_b"], f)[:depth]
    ncols = depth * 1536
    w_ada_flat = np.asarray(inp["w_ada"], f)[:depth].transpose(1, 0, 2).reshape(2048, depth * 12288)
    b_ada_flat = b_ada.reshape(depth * 12288)
    cond5 = np.concatenate([c[:4], c_ctx[None, :]], axis=0)
    if cond5.shape[0] < 5:
        cond5 = np.concatenate([np.repeat(c[:1], 4, 0), c_ctx[None, :]], axis=0)
    cond5 = np.ascontiguousarray(cond5.T.reshape(16, 128, 5).transpose(1, 0, 2))
    common = {
        "cond5": cond5,
        "w_in": np.ascontiguousarray(np.asarray(inp["w_in"], f)[:depth]),
        "bgate": np.ascontiguousarray(np.concatenate([np.asarray(inp["b_igate"], f)[:depth],
                                                      np.asarray(inp["b_fgate"], f)[:depth]], axis=-1)),
        "mhg": np.ascontiguousarray(np.asarray(inp["mh_norm_g"], f)[:depth]),
        "conv_w": np.ascontiguousarray(conv_w.reshape(depth, 3, 8, 128).transpose(0, 3, 1, 2)),
        "conv_b": np.ascontiguousarray(conv_b.reshape(depth, 8, 128).transpose(0, 2, 1)),
        "w_out": np.ascontiguousarray(np.asarray(inp["w_out"], f)[:depth]),
        "ln1_g": np.ascontiguousarray(np.asarray(inp["ln1_g"], f)[:depth]),
        "ln1_b": np.ascontiguousarray(np.asarray(inp["ln1_b"], f)[:depth]),
        "ln2_g": np.ascontiguousarray(np.asarray(inp["ln2_g"], f)[:depth]),
        "ln2_b": np.ascontiguousarray(np.asarray(inp["ln2_b"], f)[:depth]),
        "w_router": np.ascontiguousarray(np.asarray(inp["w_router"], f).reshape(16, 128, 16).transpose(1, 0, 2)),
        "b_router": np.ascontiguousarray(np.asarray(inp["b_router"], f).reshape(1, 16)),
        "w1": np.ascontiguousarray(np.asarray(inp["w1"], f)[:depth]),
        "w3": np.ascontiguousarray(np.asarray(inp["w3"], f)[:depth]),
        "w2": np.ascontiguousarray(np.asarray(inp["w2"], f)[:depth]),
        "ident": np.eye(128, dtype=f),
        "masks": np.stack([np.triu(np.ones((128, 128), f)), np.tril(np.ones((128, 128), f))]),
    }
    in_maps = []
    for r in cores:
        b, j = r // 2, r % 2
        m = dict(common)
        m["x_in"] = np.ascontiguousarray(np.concatenate([ctx[b], x[b]], axis=0))
        m["w_ada_sh"] = np.ascontiguousarray(w_ada_flat[:, r * ncols:(r + 1) * ncols])
        m["b_ada_sh"] = np.ascontiguousarray(b_ada_flat[r * ncols:(r + 1) * ncols].reshape(depth * 12, 128).T)
        sb_ = np.zeros((128, 4), f)
        sb_[:, b] = 1.0
        m["selb"] = sb_
        s = np.zeros((128, 2), f)
        s[:, j] = 1.0
        m["sel"] = s
        in_maps.append(m)
    return in_maps


def kernel(**inputs):
    nc = build(depth=4)
    in_maps = make_inputs(inputs, depth=4)
    res = run_bass_kernel_spmd(nc, in_maps, core_ids=list(range(8)))
    outs = [np.asarray(res.results[2 * b]["out"], np.float32) for b in range(4)]
    return np.stack(outs, axis=0)
```
